# Optimizing a Trainium2 kernel written in Bass

```python
import math
import jax
import jax.numpy as jnp
from jax import lax
import numpy as np

D_MODEL = 1024
BATCH = 2
SEQ = 8192
DEPTH = 1

EPS = 1e-6
D_MIX = D_MODEL

GLA_HEADS = 4
GLA_DK = 64
GLA_DV = 128
GLA_WIDTH = GLA_HEADS * GLA_DV
GLA_QK_WIDTH = GLA_HEADS * GLA_DK
GLA_GATE_RANK = 16
GLA_GATE_NORMALIZER = 16.0
GLA_CHUNK = 64

SSD_HEADS = 8
SSD_HEAD_DIM = 64
SSD_WIDTH = SSD_HEADS * SSD_HEAD_DIM
SSD_GROUPS = 2
SSD_STATE = 64
SSD_CONV = 4
SSD_CHUNK = 128
SSD_BC_WIDTH = SSD_GROUPS * SSD_STATE
SSD_CONV_DIM = SSD_WIDTH + 2 * SSD_BC_WIDTH

IN_SIZES = (GLA_QK_WIDTH, GLA_QK_WIDTH, GLA_WIDTH, GLA_GATE_RANK, GLA_WIDTH,
            SSD_WIDTH, SSD_CONV_DIM, SSD_HEADS)
IN_TOTAL = (2 * GLA_QK_WIDTH + 2 * GLA_WIDTH + GLA_GATE_RANK
            + SSD_WIDTH + SSD_CONV_DIM + SSD_HEADS)

PEER_HEADS = 8
PEER_KEYS = 128
PEER_EXPERTS = PEER_KEYS * PEER_KEYS
PEER_TOPK = 16
PEER_DQ = 256
PEER_HALF = PEER_DQ // 2
PEER_BLOCK = 128

kernel_name = "hybrid_gla_ssd_peer_layer"


def rmsnorm(x, w):
    xf = x.astype(jnp.float32)
    xf = xf * lax.rsqrt(jnp.mean(xf * xf, axis=-1, keepdims=True) + EPS)
    return xf * w.astype(jnp.float32)


def split_cols(proj):
    out, start = [], 0
    for size in IN_SIZES:
        out.append(proj[..., start:start + size])
        start += size
    return out


def gla_mixer(q, k, v, gate_lr, g_out, w_gate2, b_gate, norm_w):
    f32 = jnp.float32
    b, l, _ = q.shape
    nc, C, H = l // GLA_CHUNK, GLA_CHUNK, GLA_HEADS
    q = q.astype(f32).reshape(b, nc, C, H, GLA_DK) * (GLA_DK ** -0.5)
    k = k.astype(f32).reshape(b, nc, C, H, GLA_DK)
    v = v.astype(f32).reshape(b, nc, C, H, GLA_DV)
    log_a = jax.nn.log_sigmoid((gate_lr.astype(f32) @ w_gate2.astype(f32)
                                + b_gate.astype(f32))) / GLA_GATE_NORMALIZER
    log_a = log_a.reshape(b, nc, C, H, GLA_DK)
    G = jnp.cumsum(log_a, axis=2)
    G_last = G[:, :, -1:]
    q_in = q * jnp.exp(G)
    k_in = k * jnp.exp(-G)
    k_end = k * jnp.exp(G_last - G)
    causal = jnp.tril(jnp.ones((C, C), dtype=bool))
    att = jnp.einsum('bcihd,bcjhd->bchij', q_in, k_in)
    att = jnp.where(causal, att, 0.0)
    o_intra = jnp.einsum('bchij,bcjhv->bcihv', att, v)
    chunk_state = jnp.einsum('bcjhd,bcjhv->bchdv', k_end, v)
    chunk_decay = jnp.exp(G_last[:, :, 0])

    def step(S, inp):
        dec, st = inp
        return dec[..., None] * S + st, S

    S0 = jnp.zeros((b, H, GLA_DK, GLA_DV), f32)
    _, S_prev = lax.scan(step, S0, (jnp.moveaxis(chunk_decay, 1, 0),
                                    jnp.moveaxis(chunk_state, 1, 0)))
    S_prev = jnp.moveaxis(S_prev, 0, 1)
    o_inter = jnp.einsum('bcihd,bchdv->bcihv', q_in, S_prev)
    o = (o_intra + o_inter).reshape(b, l, H, GLA_DV)
    o = rmsnorm(o, norm_w)
    return o.reshape(b, l, GLA_WIDTH) * jax.nn.silu(g_out.astype(f32))


def causal_depthwise_conv(x, w, bias):
    c = x.shape[-1]
    out = lax.conv_general_dilated(
        x, w.astype(x.dtype)[:, None, :], window_strides=(1,),
        padding=[(SSD_CONV - 1, 0)], dimension_numbers=('NWC', 'WIO', 'NWC'),
        feature_group_count=c)
    return out + bias.astype(x.dtype)


def ssd_mixer(z, xbc, dt_raw, conv_w, conv_b, dt_bias, a_log, d_skip, norm_w):
    f32 = jnp.float32
    b, l, _ = z.shape
    nc, C = l // SSD_CHUNK, SSD_CHUNK
    G, J, P, N = SSD_GROUPS, SSD_HEADS // SSD_GROUPS, SSD_HEAD_DIM, SSD_STATE
    xbc = jax.nn.silu(causal_depthwise_conv(xbc.astype(f32), conv_w, conv_b))
    xs = xbc[..., :SSD_WIDTH]
    Bm = xbc[..., SSD_WIDTH:SSD_WIDTH + SSD_BC_WIDTH].reshape(b, nc, C, G, N)
    Cm = xbc[..., SSD_WIDTH + SSD_BC_WIDTH:].reshape(b, nc, C, G, N)
    X = xs.reshape(b, nc, C, G, J, P)
    dt = jax.nn.softplus(dt_raw.astype(f32) + dt_bias.astype(f32)).reshape(b, nc, C, G, J)
    A = -jnp.exp(a_log.astype(f32)).reshape(G, J)
    a = dt * A
    a_cum = jnp.cumsum(a, axis=2)
    seg = a_cum[:, :, :, None] - a_cum[:, :, None]
    causal = jnp.tril(jnp.ones((C, C), dtype=bool))[:, :, None, None]
    Lmat = jnp.exp(jnp.where(causal, seg, -jnp.inf))
    Xdt = X * dt[..., None]
    scores = jnp.einsum('bclgn,bcsgn->bclsg', Cm, Bm)
    y_diag = jnp.einsum('bclsg,bclsgj,bcsgjp->bclgjp', scores, Lmat, Xdt)
    decay_to_end = jnp.exp(a_cum[:, :, -1:] - a_cum)
    states = jnp.einsum('bclgn,bclgj,bclgjp->bcgjpn', Bm, decay_to_end, Xdt)
    chunk_decay = jnp.exp(a_cum[:, :, -1])

    def step(S, inp):
        dec, st = inp
        return dec[..., None, None] * S + st, S

    S0 = jnp.zeros((b, G, J, P, N), f32)
    _, S_prev = lax.scan(step, S0, (jnp.moveaxis(chunk_decay, 1, 0),
                                    jnp.moveaxis(states, 1, 0)))
    S_prev = jnp.moveaxis(S_prev, 0, 1)
    y_off = jnp.einsum('bclgn,bcgjpn,bclgj->bclgjp', Cm, S_prev, jnp.exp(a_cum))
    y = y_diag + y_off + X * d_skip.astype(f32).reshape(G, J)[..., None]
    y = y.reshape(b, l, SSD_WIDTH) * jax.nn.silu(z.astype(f32))
    y = y.reshape(b, l, G, SSD_WIDTH // G)
    y = y * lax.rsqrt(jnp.mean(y * y, axis=-1, keepdims=True) + EPS)
    return y.reshape(b, l, SSD_WIDTH) * norm_w.astype(f32)


def peer_ffn(xn, w_query, sub_keys, u, v):
    f32 = jnp.float32
    b, l, d = xn.shape
    T = b * l
    H, K = PEER_HEADS, PEER_TOPK
    xt = xn.reshape(T, d).astype(u.dtype)
    q = (xt @ w_query).reshape(T, H, 2, PEER_HALF)
    s = jnp.einsum('thpd,hpkd->thpk', q, sub_keys).astype(f32)
    top_s, top_i = lax.top_k(s, K)
    cand_s = (top_s[:, :, 0, :, None] + top_s[:, :, 1, None, :]).reshape(T, H, K * K)
    cand_i = (top_i[:, :, 0, :, None] * PEER_KEYS + top_i[:, :, 1, None, :]).reshape(T, H, K * K)
    best_s, best_pos = lax.top_k(cand_s, K)
    expert_idx = jnp.take_along_axis(cand_i, best_pos, axis=-1)
    gate = jax.nn.softmax(best_s, axis=-1)
    nb = T // PEER_BLOCK
    xb = xt.reshape(nb, PEER_BLOCK, d)
    ib = expert_idx.reshape(nb, PEER_BLOCK, H * K)
    gb = gate.reshape(nb, PEER_BLOCK, H * K)

    def block(args):
        x_blk, i_blk, g_blk = args
        act = jnp.einsum('td,ted->te', x_blk, u[i_blk]).astype(f32)
        hid = jax.nn.gelu(act, approximate=False) * g_blk
        return jnp.einsum('te,ted->td', hid.astype(v.dtype), v[i_blk])

    out = lax.map(block, (xb, ib, gb))
    return out.reshape(b, l, d)


def setup_inputs(seed: int = 0) -> dict:
    key = jax.random.key(seed)
    ks = jax.random.split(key, 24)
    f32 = jnp.float32
    nrm = lambda k, shape, scale: jax.random.normal(k, shape, f32) * scale
    dt0 = jnp.exp(jax.random.uniform(ks[10], (DEPTH, SSD_HEADS), f32,
                                     math.log(1e-3), math.log(1e-1)))
    return {
        "x": jax.random.normal(ks[0], (BATCH, SEQ, D_MODEL), f32),
        "norm_mix_w": 1.0 + nrm(ks[1], (DEPTH, D_MODEL), 0.02),
        "w_in": nrm(ks[2], (DEPTH, D_MODEL, IN_TOTAL), D_MODEL ** -0.5),
        "gla_w_gate2": nrm(ks[3], (DEPTH, GLA_GATE_RANK, GLA_QK_WIDTH), GLA_GATE_RANK ** -0.5),
        "gla_b_gate": nrm(ks[4], (DEPTH, GLA_QK_WIDTH), 0.1),
        "gla_norm_w": 1.0 + nrm(ks[5], (DEPTH, GLA_DV), 0.02),
        "ssd_conv_w": nrm(ks[6], (DEPTH, SSD_CONV, SSD_CONV_DIM), SSD_CONV ** -0.5),
        "ssd_conv_b": nrm(ks[7], (DEPTH, SSD_CONV_DIM), 0.02),
        "ssd_dt_bias": dt0 + jnp.log(-jnp.expm1(-dt0)),
        "ssd_a_log": jnp.log(jax.random.uniform(ks[8], (DEPTH, SSD_HEADS), f32, 1.0, 16.0)),
        "ssd_d": 1.0 + nrm(ks[9], (DEPTH, SSD_HEADS), 0.1),
        "ssd_norm_w": 1.0 + nrm(ks[11], (DEPTH, SSD_WIDTH), 0.02),
        "w_out": nrm(ks[12], (DEPTH, D_MIX, D_MODEL), D_MIX ** -0.5),
        "norm_ffn_w": 1.0 + nrm(ks[13], (DEPTH, D_MODEL), 0.02),
        "peer_w_query": nrm(ks[14], (DEPTH, D_MODEL, PEER_HEADS * PEER_DQ), D_MODEL ** -0.5),
        "peer_sub_keys": nrm(ks[15], (DEPTH, PEER_HEADS, 2, PEER_KEYS, PEER_HALF), PEER_HALF ** -0.5),
        "peer_u": nrm(ks[16], (DEPTH, PEER_EXPERTS, D_MODEL), D_MODEL ** -0.5),
        "peer_v": nrm(ks[17], (DEPTH, PEER_EXPERTS, D_MODEL), PEER_HEADS ** -0.5),
        "norm_final_w": 1.0 + nrm(ks[18], (D_MODEL,), 0.02),
    }


def reference(x, norm_mix_w, w_in, gla_w_gate2, gla_b_gate, gla_norm_w,
              ssd_conv_w, ssd_conv_b, ssd_dt_bias, ssd_a_log, ssd_d, ssd_norm_w,
              w_out, norm_ffn_w, peer_w_query, peer_sub_keys, peer_u, peer_v,
              norm_final_w):
    h = x
    for i in range(DEPTH):
        n = rmsnorm(h, norm_mix_w[i]).astype(w_in.dtype)
        proj = n @ w_in[i]
        q, k, v, gate_lr, g_out, z, xbc, dt_raw = split_cols(proj)
        o_gla = gla_mixer(q, k, v, gate_lr, g_out, gla_w_gate2[i], gla_b_gate[i], gla_norm_w[i])
        o_ssd = ssd_mixer(z, xbc, dt_raw, ssd_conv_w[i], ssd_conv_b[i], ssd_dt_bias[i],
                          ssd_a_log[i], ssd_d[i], ssd_norm_w[i])
        mixed = jnp.concatenate([o_gla, o_ssd], axis=-1).astype(w_out.dtype)
        h = h + (mixed @ w_out[i]).astype(h.dtype)
        n2 = rmsnorm(h, norm_ffn_w[i])
        h = h + peer_ffn(n2, peer_w_query[i], peer_sub_keys[i], peer_u[i], peer_v[i]).astype(h.dtype)
    return rmsnorm(h, norm_final_w).astype(x.dtype)
```

```python
import contextlib
import numpy as np
import concourse.bass as bass
import concourse.mybir as mybir
from concourse.bass_utils import run_bass_kernel_spmd

F32 = mybir.dt.float32
BF16 = mybir.dt.bfloat16
U32 = mybir.dt.uint32
I32 = mybir.dt.int32
AF = mybir.ActivationFunctionType
ALU = mybir.AluOpType
AX = mybir.AxisListType

SEM_CHUNK = 4096
STRICT_SAME_ENGINE = True


class Buf:
    def __init__(self, ap, name=""):
        self.ap = ap
        self.name = name
        self.last_write = None
        self.readers = []
        self.is_psum = False

    def __getitem__(self, key):
        return View(self, self.ap[key])

    def v(self, ap):
        return View(self, ap)

    def sub(self, key, name=""):
        return Buf(self.ap[key], name or self.name)


class View:
    def __init__(self, buf, ap):
        self.buf = buf
        self.ap = ap

    def __getitem__(self, key):
        return View(self.buf, self.ap[key])

    def re(self, s, **kw):
        return View(self.buf, self.ap.rearrange(s, **kw))

    def bc(self, shape):
        return View(self.buf, self.ap.broadcast_to(shape))

    def us(self, axis):
        return View(self.buf, self.ap.unsqueeze(axis))

    def bitcast(self, dt):
        return View(self.buf, self.ap.bitcast(dt))


class EngState:
    def __init__(self, ctx, name, handle):
        self.ctx = ctx
        self.name = name
        self.h = handle
        self.count = 0
        self.sems = []
        self.seen = {}

    def sem_for(self, n):
        idx = (n - 1) // SEM_CHUNK
        while len(self.sems) <= idx:
            self.sems.append(self.ctx.new_sem(f"{self.name}{len(self.sems)}"))
        return self.sems[idx], (n - 1) % SEM_CHUNK + 1


class Ctx:
    def __init__(self, nc):
        self.nc = nc
        self.es = contextlib.ExitStack()
        self.nsem = 0
        self.pe = EngState(self, "pe", nc.tensor)
        self.act = EngState(self, "act", nc.scalar)
        self.dve = EngState(self, "dve", nc.vector)
        self.pool = EngState(self, "pool", nc.gpsimd)
        self.sp = EngState(self, "sp", nc.sync)
        self.engs = [self.pe, self.act, self.dve, self.pool, self.sp]
        self.dma_rings = {}
        self.all_dma_tokens = []

    def new_sem(self, name):
        self.nsem += 1
        return self.es.enter_context(self.nc.semaphore(f"s_{name}_{self.nsem}"))

    def sbuf(self, name, shape, dt, es=None):
        t = (es or self.es).enter_context(self.nc.sbuf_tensor(name, list(shape), dt))
        return Buf(t[:] if hasattr(t, "__getitem__") else t, name)

    def psum(self, name, shape, dt, es=None):
        t = (es or self.es).enter_context(self.nc.psum_tensor(name, list(shape), dt))
        b = Buf(t[:], name)
        b.is_psum = True
        return b

    def dram(self, name, shape, dt, kind):
        t = self.nc.dram_tensor(name, list(shape), dt, kind=kind)
        return Buf(t.ap(), name)

    def _wait(self, eng, tok):
        if tok is None:
            return
        if tok[0] == "e":
            _, src, n = tok
            if src is eng:
                return
            if eng.seen.get(src.name, 0) >= n:
                return
            sem, val = src.sem_for(n)
            eng.h.wait_ge(sem, val)
            eng.seen[src.name] = n
        else:
            _, sid, sem, val = tok
            key = ("d", sid)
            if eng.seen.get(key, 0) >= val:
                return
            eng.h.wait_ge(sem, val)
            eng.seen[key] = val

    def _deps(self, eng, reads, writes, same_engine_raw=True):
        toks = []
        for b in reads:
            if b.last_write is not None:
                toks.append(b.last_write)
            if b.is_psum:
                toks.extend(b.readers)
        for b in writes:
            if b.last_write is not None:
                toks.append(b.last_write)
            toks.extend(b.readers)
        for tok in toks:
            if tok[0] == "e" and tok[1] is eng:
                continue
            self._wait(eng, tok)
        if eng is not self.pe and same_engine_raw:
            mx = 0
            for b in reads:
                lw = b.last_write
                if lw is not None and lw[0] == "e" and lw[1] is eng:
                    mx = max(mx, lw[2])
            if STRICT_SAME_ENGINE:
                for b in writes:
                    lw = b.last_write
                    if lw is not None and lw[0] == "e" and lw[1] is eng:
                        mx = max(mx, lw[2])
                    for r in b.readers:
                        if r[0] == "e" and r[1] is eng:
                            mx = max(mx, r[2])
            if mx > eng.seen.get(eng.name, 0):
                sem, val = eng.sem_for(mx)
                eng.h.wait_ge(sem, val)
                eng.seen[eng.name] = mx

    def _commit(self, tok, reads, writes):
        for b in writes:
            b.last_write = tok
            b.readers = []
        for b in reads:
            if b not in writes:
                b.readers.append(tok)
                if len(b.readers) > 64:
                    b.readers = self._prune(b.readers)

    @staticmethod
    def _prune(readers):
        best = {}
        out = []
        for t in readers:
            if t[0] == "e":
                k = t[1].name
                if k not in best or best[k][2] < t[2]:
                    best[k] = t
            else:
                out.append(t)
        return out + list(best.values())

    def op(self, eng, fn, reads, writes):
        reads = [r.buf if isinstance(r, View) else r for r in reads]
        writes = [w.buf if isinstance(w, View) else w for w in writes]
        self._deps(eng, reads, writes)
        ins = fn()
        eng.count += 1
        sem, val = eng.sem_for(eng.count)
        ins.then_inc(sem, 1)
        self._commit(("e", eng, eng.count), reads, writes)
        return ins

    def dma(self, eng, out, in_, ring=8, **kw):
        rb, wb = in_.buf, out.buf
        self._deps(eng, [rb], [wb], same_engine_raw=False)
        key = eng.name
        if key not in self.dma_rings:
            self.dma_rings[key] = {"n": 0, "sems": [], "last": {}}
        R = self.dma_rings[key]
        slot = R["n"] % ring
        while len(R["sems"]) <= slot:
            R["sems"].append(self.new_sem(f"dma_{key}{len(R['sems'])}"))
        sem = R["sems"][slot]
        val = 16 * (R["n"] // ring + 1)
        sid = (key, slot)
        if val > 16:
            self._wait(eng, ("d", sid, sem, val - 16))
        R["n"] += 1
        ins = eng.h.dma_start(out=out.ap, in_=in_.ap, **kw)
        ins.then_inc(sem, 16)
        tok = ("d", sid, sem, val)
        self._commit(tok, [rb], [wb])
        self.all_dma_tokens.append(tok)
        return tok

    def barrier(self):
        for e in self.engs:
            for s in self.engs:
                if s is not e and s.count > 0:
                    self._wait(e, ("e", s, s.count))
            for tok in self._last_dma_tokens():
                self._wait(e, tok)

    def _last_dma_tokens(self):
        last = {}
        for tok in self.all_dma_tokens:
            last[tok[1]] = tok
        self.all_dma_tokens = list(last.values())
        return self.all_dma_tokens

    def finish(self, toks):
        for t in toks:
            self._wait(self.sp, t)

    def mm(self, out, lhsT, rhs, start=True, stop=True, **kw):
        return self.op(self.pe, lambda: self.nc.tensor.matmul(out.ap, lhsT.ap, rhs.ap, start=start, stop=stop, **kw),
                       [lhsT, rhs] + ([] if start else []), [out])

    def tr(self, out, in_, ident):
        return self.op(self.pe, lambda: self.nc.tensor.transpose(out.ap, in_.ap, ident.ap), [in_, ident], [out])

    def actv(self, out, in_, func, bias=None, scale=None, accum=None, eng=None):
        reads = [in_]
        kw = {}
        if bias is not None:
            if isinstance(bias, View):
                reads.append(bias); kw["bias"] = bias.ap
            else:
                kw["bias"] = bias
        if scale is not None:
            if isinstance(scale, View):
                reads.append(scale); kw["scale"] = scale.ap
            else:
                kw["scale"] = scale
        writes = [out]
        if accum is not None:
            writes.append(accum); kw["accum_out"] = accum.ap
        return self.op(self.act, lambda: self.nc.scalar.activation(out.ap, in_.ap, func, **kw), reads, writes)

    def _ve(self, eng):
        return eng or self.dve

    def tt(self, out, a, b, op, eng=None):
        e = self._ve(eng)
        return self.op(e, lambda: e.h.tensor_tensor(out.ap, a.ap, b.ap, op), [a, b], [out])

    def ts(self, out, a, s1, s2, op0, op1=None, accum=None, eng=None):
        e = self._ve(eng)
        reads = [a]
        v1 = s1.ap if isinstance(s1, View) else s1
        v2 = s2.ap if isinstance(s2, View) else s2
        if isinstance(s1, View): reads.append(s1)
        if isinstance(s2, View): reads.append(s2)
        writes = [out]
        kw = {}
        if op1 is not None: kw["op1"] = op1
        if accum is not None:
            kw["accum_out"] = accum.ap; writes.append(accum)
        return self.op(e, lambda: e.h.tensor_scalar(out.ap, a.ap, v1, v2, op0, **kw), reads, writes)

    def stt(self, out, a, s, b, op0, op1, accum=None, eng=None):
        e = self._ve(eng)
        reads = [a, b]
        sv = s.ap if isinstance(s, View) else s
        if isinstance(s, View): reads.append(s)
        writes = [out]
        kw = {}
        if accum is not None:
            kw["accum_out"] = accum.ap; writes.append(accum)
        return self.op(e, lambda: e.h.scalar_tensor_tensor(out.ap, a.ap, sv, b.ap, op0, op1, **kw), reads, writes)

    def copy(self, out, in_, eng=None):
        e = self._ve(eng)
        return self.op(e, lambda: e.h.tensor_copy(out.ap, in_.ap), [in_], [out])

    def memset(self, out, val, eng=None):
        e = self._ve(eng)
        return self.op(e, lambda: e.h.memset(out.ap, val), [], [out])

    def reduce(self, out, in_, op, axis=AX.X, eng=None):
        e = self._ve(eng)
        return self.op(e, lambda: e.h.tensor_reduce(out.ap, in_.ap, axis, op), [in_], [out])

EPS = 1e-6
D = 1024
IN_TOTAL = 2840
C_Q, C_K, C_V, C_LR, C_GO, C_Z, C_XBC, C_DT = 0, 256, 512, 1024, 1040, 1552, 2064, 2832
NEXP_GROUP = 512
NG = 16384 // NEXP_GROUP


class _Stop(Exception):
    pass


def build_program(NPRE, NOWN, stage=99, stop=None):
    def cp(k):
        if stop is not None and k == stop:
            raise _Stop()
    try:
        return _build_program(NPRE, NOWN, stage, cp)
    except _Stop as e:
        c = _LAST[0]
        c.barrier(); c.finish(c._last_dma_tokens())
        return c.nc


_LAST = [None]


def _build_program(NPRE, NOWN, stage, cp):
    nc = bass.Bass("TRN2", target_bir_lowering=False)
    c = Ctx(nc)
    _LAST[0] = c
    NT = NPRE + NOWN
    TOK = NOWN * 128
    xin = c.dram("xin", [NT * 128, D], F32, "ExternalInput")
    maskd = c.dram("maskd", [128, NT], F32, "ExternalInput")
    constd = c.dram("constd", [128, 8 * 128], F32, "ExternalInput")
    ppd = c.dram("ppd", [128, 46], F32, "ExternalInput")
    bcd = c.dram("bcd", [128, 24 + 512 + 512 + 1024], F32, "ExternalInput")
    wg2bd = c.dram("wg2bd", [17, 256], F32, "ExternalInput")
    w_in = c.dram("w_in", [D, IN_TOTAL], F32, "ExternalInput")
    w_out = c.dram("w_out", [D, D], F32, "ExternalInput")
    w_q = c.dram("w_q", [D, 2048], F32, "ExternalInput")
    skTd = c.dram("skTd", [128, 16 * 128], F32, "ExternalInput")
    uTd = c.dram("uTd", [D, 16384], F32, "ExternalInput")
    vd = c.dram("vd", [16384, D], F32, "ExternalInput")
    repd = c.dram("repd", [24, 256], F32, "ExternalInput")
    y = c.dram("y", [TOK, D], F32, "ExternalOutput")
    hs = c.dram("hs", [TOK, D], F32, "Internal" if stage > 1 else "ExternalOutput")
    if stage <= 1:
        dbg = c.dram("dbg", [TOK, D], BF16, "ExternalOutput")
    if stage > 2:
        Gd = c.dram("Gd", [NG * 128, TOK * 4], BF16, "Internal")

    ubd_t = nc.dram_tensor("ubd", [D, 16384], BF16, kind="Internal").ap()
    vbd_t = nc.dram_tensor("vbd", [16384, D], BF16, kind="Internal").ap()
    ubd = [Buf(ubd_t[:, g * 512:(g + 1) * 512], f"ubd{g}") for g in range(NG)]
    vbd = [Buf(vbd_t[g * 512:(g + 1) * 512, :], f"vbd{g}") for g in range(NG)]
    conv_jobs = [("u", g) for g in range(NG)] + [("v", g) for g in range(NG)]

    def issue_conv():
        if not conv_jobs or stage <= 1:
            return
        kind, g = conv_jobs.pop(0)
        if kind == "u":
            c.dma(c.pool, ubd[g][:], uTd[:, g * 512:(g + 1) * 512])
        else:
            c.dma(c.pool, vbd[g][:], vd[g * 512:(g + 1) * 512, :])

    const = c.sbuf("const", [128, 8, 128], F32)
    ident_f = const[:, 0, :]; TriNeg = const[:, 1, :]; StrictNeg = const[:, 2, :]; gmask = const[:, 3, :]
    TriIncl = const[:, 4, :]; StrictGT = const[:, 5, :]; Ones = const[:, 6, :]; iota_f = const[:, 7, :]
    ident_b = c.sbuf("ident_b", [128, 128], BF16)
    pp = c.sbuf("pp", [128, 46], F32)
    bcs = c.sbuf("bcs", [128, 24 + 512 + 512 + 1024], F32)
    maskt = c.sbuf("maskt", [128, NT], F32)
    Abc = c.sbuf("Abc", [128, 8], F32)
    n2T = c.sbuf("n2T", [128, 8, TOK], BF16)
    banks = [c.psum(f"bank{i}", [128, 512], F32) for i in range(8)]
    bctr = [0]

    def nb():
        b = banks[bctr[0] % 8]
        bctr[0] += 1
        return b

    c.dma(c.sp, const[:].re("p a b -> p (a b)"), constd[:])
    c.dma(c.sp, pp[:], ppd[:])
    c.dma(c.sp, bcs[:], bcd[:])
    c.dma(c.sp, maskt[:], maskd[:])
    c.copy(ident_b[:], ident_f)
    c.actv(Abc[:], bcs[:, 8:16], AF.Exp)
    c.ts(Abc[:], Abc[:], -1.0, None, ALU.mult)
    nmw = pp[:, 0:8]; nfw = pp[:, 8:16]; cw = pp[:, 16:40].re("p (i k) -> p i k", k=4); cb = pp[:, 40:46]
    dtb = bcs[:, 0:8]; dsk = bcs[:, 16:24]; gnw = bcs[:, 24:536]; snw = bcs[:, 536:1048]; fnw = bcs[:, 1048:2072]

    def rmsnorm_T(xt, wcol, outT, scr):
        ss, junk, xsb = scr
        c.memset(ss[:, 0:1], 0.0)
        c.actv(junk[:], xt, AF.Square, accum=ss[:, 0:1])
        c.ts(ss[:, 1:2], ss[:, 0:1], 1.0 / D, EPS, ALU.mult, ALU.add)
        c.actv(ss[:, 2:3], ss[:, 1:2], AF.Ln)
        c.actv(ss[:, 3:4], ss[:, 2:3], AF.Exp, scale=-0.5)
        c.ts(xsb[:], xt, ss[:, 3:4], None, ALU.mult)
        pb = banks[0]
        pbb = pb[:].bitcast(BF16)
        for dc in range(8):
            c.tr(pbb[:, dc * 128:(dc + 1) * 128], xsb[:, dc * 128:(dc + 1) * 128], ident_b[:])
        c.tt(outT, pbb.re("p (a b) -> p a b", b=128), wcol.us(2).bc([128, 8, 128]), ALU.mult)

    esA = contextlib.ExitStack()
    Win = c.sbuf("Win", [128, 8, IN_TOTAL], BF16, es=esA)
    Wout = c.sbuf("Wout", [128, 8, D], BF16, es=esA)
    wg2b = c.sbuf("wg2b", [17, 256], F32, es=esA)
    for dc in range(8):
        c.dma(c.pool, Win[:, dc, :], w_in[dc * 128:(dc + 1) * 128, :])
    c.dma(c.pool, Wout[:], w_out[:].re("(c p) n -> p c n", p=128))
    c.dma(c.sp, wg2b[:], wg2bd[:])
    xb = [c.sbuf(f"xb{i}", [128, D], F32, es=esA) for i in range(2)]
    ss = c.sbuf("ss", [128, 4], F32, es=esA)
    junk = c.sbuf("junk", [128, D], F32, es=esA)
    xsb = c.sbuf("xsb", [128, D], BF16, es=esA)
    nT = c.sbuf("nT", [128, 8, 128], BF16, es=esA)
    lr1 = c.sbuf("lr1", [17, 128], F32, es=esA)
    Lt = c.sbuf("Lt", [128, 256], F32, es=esA)
    eG = c.sbuf("eG", [128, 256], F32, es=esA)
    emG = c.sbuf("emG", [128, 256], F32, es=esA)
    eGe = c.sbuf("eGe", [128, 256], F32, es=esA)
    dec = c.sbuf("dec", [128, 2, 2], F32, es=esA)
    qTin = c.sbuf("qTin", [128, 2, 128], BF16, es=esA)
    kTin = c.sbuf("kTin", [128, 2, 128], BF16, es=esA)
    kend = c.sbuf("kend", [128, 256], BF16, es=esA)
    vsb = c.sbuf("vsb", [128, 512], BF16, es=esA)
    gsil = c.sbuf("gsil", [128, 512], BF16, es=esA)
    zsil = c.sbuf("zsil", [128, 512], BF16, es=esA)
    attm = c.sbuf("attm", [128, 4, 128], BF16, es=esA)
    S32 = c.sbuf("S32", [128, 2, 128], F32, es=esA)
    Sb = [c.sbuf(f"Sb{i}", [128, 2, 128], BF16, es=esA) for i in range(2)]
    gs = c.sbuf("gs", [128, 8], F32, es=esA)
    og = c.sbuf("og", [128, 512], F32, es=esA)
    mixed = c.sbuf("mixed", [128, 1024], BF16, es=esA)
    xr = c.sbuf("xr", [128, 6, 131], BF16, es=esA)
    cv = c.sbuf("cv", [128, 6, 128], F32, es=esA)
    cvt = c.sbuf("cvt", [128, 6, 128], F32, es=esA)
    xc = c.sbuf("xc", [128, 6, 128], BF16, es=esA)
    xtok = c.sbuf("xtok", [128, 640], BF16, es=esA)
    d1 = c.sbuf("d1", [128, 8], F32, es=esA)
    d2 = c.sbuf("d2", [128, 8], F32, es=esA)
    dtt = c.sbuf("dtt", [128, 8], F32, es=esA)
    at = c.sbuf("at", [128, 8], F32, es=esA)
    ec = c.sbuf("ec", [128, 24], F32, es=esA)
    Xdt = c.sbuf("Xdt", [128, 512], BF16, es=esA)
    Xde = c.sbuf("Xde", [128, 512], BF16, es=esA)
    sm = c.sbuf("sm", [128, 2, 128], F32, es=esA)
    Rb = c.sbuf("Rb", [128, 8, 128], F32, es=esA)
    LT = c.sbuf("LT", [128, 8, 128], F32, es=esA)
    MT = c.sbuf("MT", [128, 8, 128], BF16, es=esA)
    yb = c.sbuf("yb", [128, 512], F32, es=esA)
    ytmp = c.sbuf("ytmp", [128, 512], F32, es=esA)
    Ss32 = c.sbuf("Ss32", [128, 256], F32, es=esA)
    Ssb = c.sbuf("Ssb", [128, 256], BF16, es=esA)
    dsel = c.sbuf("dsel", [128, 4], F32, es=esA)
    mT = c.sbuf("mT", [128, 8, 128], BF16, es=esA)
    hbuf = c.sbuf("hbuf", [128, D], F32, es=esA)

    c.memset(lr1[:], 1.0)
    c.memset(S32[:], 0.0)
    c.memset(Sb[0][:], 0.0)
    c.memset(Ss32[:], 0.0)
    c.memset(Ssb[:], 0.0)
    c.memset(xr[:], 0.0)

    def fm(outv, col0, M):
        for dc in range(8):
            c.mm(outv, Win[:, dc, col0:col0 + M], nT[:, dc, :], start=(dc == 0), stop=(dc == 7))

    def tm(outv, col0, N):
        for dc in range(8):
            c.mm(outv, nT[:, dc, :], Win[:, dc, col0:col0 + N], start=(dc == 0), stop=(dc == 7))

    xsb2 = [xsb, c.sbuf("xsbB", [128, D], BF16, es=esA)]
    ss2 = [ss, c.sbuf("ssB", [128, 4], F32, es=esA)]
    nT2 = [nT, c.sbuf("nTB", [128, 8, 128], BF16, es=esA)]
    lr12 = [lr1, c.sbuf("lr1B", [17, 128], F32, es=esA)]
    vsb2 = [vsb, c.sbuf("vsbB", [128, 512], BF16, es=esA)]
    gsil2 = [gsil, c.sbuf("gsilB", [128, 512], BF16, es=esA)]
    zsil2 = [zsil, c.sbuf("zsilB", [128, 512], BF16, es=esA)]
    xr2 = [xr, c.sbuf("xrB", [128, 6, 131], BF16, es=esA)]
    qks2 = [c.sbuf(f"qks{i}", [128, 512], F32, es=esA) for i in range(2)]
    ktok2 = [c.sbuf(f"ktok{i}", [128, 256], F32, es=esA) for i in range(2)]
    d12 = [d1, c.sbuf("d1B", [128, 8], F32, es=esA)]
    ssR = c.sbuf("ssR", [128, 4], F32, es=esA)
    xsbR = c.sbuf("xsbR", [128, D], BF16, es=esA)
    c.memset(lr12[1][:], 1.0)
    diagW = c.sbuf("diagW", [128, 6, 5, 128], BF16, es=esA)
    ones_b = c.sbuf("ones_b", [128, 128], BF16, es=esA)
    c.memset(ones_b[:], 1.0)
    for i in range(6):
        for k in range(4):
            c.ts(diagW[:, i, k, :], ident_f, cw[:, i, k:k + 1], None, ALU.mult)
        c.ts(diagW[:, i, 4, :], ident_f, cb[:, i:i + 1], None, ALU.mult)
    c.memset(xr2[1][:], 0.0)

    def stage1(t):
        own = t >= NPRE
        par = t % 2
        xt = xb[par]
        nTp = nT2[par]
        c.dma(c.sp, xt[:], xin[t * 128:(t + 1) * 128, :])
        issue_conv()
        sst, xs_ = ss2[par], xsb2[par]
        c.memset(sst[:, 0:1], 0.0)
        c.actv(junk[:], xt[:], AF.Square, accum=sst[:, 0:1])
        c.ts(sst[:, 1:2], sst[:, 0:1], 1.0 / D, EPS, ALU.mult, ALU.add)
        c.actv(sst[:, 2:3], sst[:, 1:2], AF.Ln)
        c.actv(sst[:, 3:4], sst[:, 2:3], AF.Exp, scale=-0.5)
        c.actv(xs_[:], xt[:], AF.Copy, scale=sst[:, 3:4])
        cp(80) if t == 0 else None
        yield
        pbb = banks[0][:].bitcast(BF16)
        for dc in range(8):
            c.tr(pbb[:, dc * 128:(dc + 1) * 128], xs_[:, dc * 128:(dc + 1) * 128], ident_b[:])
        c.tt(nTp[:], pbb.re("p (a b) -> p a b", b=128), nmw.us(2).bc([128, 8, 128]), ALU.mult)
        cp(81) if t == 0 else None
        yield

        def fm(outv, col0, M):
            for dc in range(8):
                c.mm(outv, Win[:, dc, col0:col0 + M], nTp[:, dc, :], start=(dc == 0), stop=(dc == 7))

        def tm(outv, col0, N):
            for dc in range(8):
                c.mm(outv, nTp[:, dc, :], Win[:, dc, col0:col0 + N], start=(dc == 0), stop=(dc == 7))

        xrp = xr2[par]
        c.copy(xrp[:, :, 0:3], xr2[1 - par][:, :, 128:131], eng=c.pool)
        pb = banks[1]
        for i in range(4):
            fm(pb[:, i * 128:(i + 1) * 128], C_XBC + i * 128, 128)
        c.actv(xrp[:, 0:4, 3:131], pb[:].re("p (a b) -> p a b", b=128), AF.Copy)
        cp(82) if t == 0 else None
        yield
        pb = banks[2]
        for i in range(2):
            fm(pb[:, i * 128:(i + 1) * 128], C_XBC + 512 + i * 128, 128)
        fm(pb[0:16, 256:384], C_LR, 16)
        c.actv(xrp[:, 4:6, 3:131], pb[:, 0:256].re("p (a b) -> p a b", b=128), AF.Copy)
        c.actv(lr12[par][0:16, :], pb[0:16, 256:384], AF.Copy)
        cp(83) if t == 0 else None
        yield
        pb = banks[1]
        tm(pb[:, 0:512], C_V, 512)
        c.actv(vsb2[par][:], pb[:, 0:512], AF.Copy)
        cp(84) if t == 0 else None
        yield
        pb = banks[2]
        tm(pb[:, 0:256], C_K, 256)
        tm(pb[:, 256:264], C_DT, 8)
        c.copy(ktok2[par][:], pb[:, 0:256])
        c.tt(d12[par][:], pb[:, 256:264], dtb, ALU.add)
        cp(85) if t == 0 else None
        yield
        if own:
            pb = banks[1]
            fm(pb[:, 0:128], C_Q, 128)
            fm(pb[:, 128:256], C_Q + 128, 128)
            fm(pb[:, 256:384], C_K, 128)
            fm(pb[:, 384:512], C_K + 128, 128)
            c.copy(qks2[par][:], pb[:])
            cp(86) if t == 0 else None
            yield
            pb = banks[2]
            tm(pb[:, 0:512], C_GO, 512)
            c.actv(gsil2[par][:], pb[:], AF.Silu)
            cp(87) if t == 0 else None
            yield
            pb = banks[1]
            tm(pb[:, 0:512], C_Z, 512)
            c.actv(zsil2[par][:], pb[:], AF.Silu)
            cp(88) if t == 0 else None
            yield

    def stage2_prefix(t, pump):
        par = t % 2
        lr1 = lr12[par]; vsb = vsb2[par]; xr = xr2[par]; ktok = ktok2[par]; d1 = d12[par]
        pCV = [banks[3], banks[5]]
        for i in range(6):
            dst = pCV[0][:, i * 128:(i + 1) * 128] if i < 4 else pCV[1][:, (i - 4) * 128:(i - 3) * 128]
            for k in range(4):
                c.mm(dst, diagW[:, i, k, :], xr[:, i, k:k + 128], start=(k == 0), stop=False)
            c.mm(dst, diagW[:, i, 4, :], ones_b[:], start=False, stop=True)
        pZG = banks[4]
        c.mm(pZG[:, 0:256], lr1[0:17, :], wg2b[0:17, :])
        c.actv(xc[:, 0:4, :].re("p a b -> p (a b)"), pCV[0][:], AF.Silu)
        c.actv(xc[:, 4:6, :].re("p a b -> p (a b)"), pCV[1][:, 0:256], AF.Silu)
        c.actv(Lt[:], pZG[:, 0:256], AF.Exp, scale=-1.0)
        c.actv(Lt[:], Lt[:], AF.Ln, bias=1.0)
        c.stt(d2[:], d1[:], -1.0, d1[:], ALU.mult, ALU.max)
        pump()
        pTRb = banks[0][:].bitcast(BF16)
        for i in range(5):
            c.tr(pTRb[:, i * 128:(i + 1) * 128], xc[:, i, :], ident_b[:])
        pGT = banks[6]
        for hp in range(2):
            c.mm(pGT[:, hp * 128:(hp + 1) * 128], Lt[:, hp * 128:(hp + 1) * 128], TriNeg)
        c.mm(pGT[:, 256:512], StrictNeg, Lt[:, 0:256])
        c.actv(d2[:], d2[:], AF.Exp, scale=-1.0)
        c.actv(d2[:], d2[:], AF.Ln, bias=1.0)
        c.actv(xtok[:], pTRb[:, 0:640], AF.Copy)
        c.actv(dec[:], pGT[:, 0:256].re("p (a b) -> p a b", b=128)[:, :, 63::64], AF.Exp)
        c.actv(eGe[:], pGT[:, 256:512], AF.Exp)
        pump()
        c.stt(dtt[:], d1[:], 0.0, d2[:], ALU.max, ALU.add)
        c.ts(dtt[:], dtt[:], maskt[:, t:t + 1], None, ALU.mult)
        c.tt(at[:], dtt[:], Abc[:], ALU.mult)
        c.tt(kend[:], ktok[:], eGe[:], ALU.mult)
        c.tt(Xdt[:].re("p (a b) -> p a b", b=64), xtok[:, 0:512].re("p (a b) -> p a b", b=64),
             dtt[:].us(2).bc([128, 8, 64]), ALU.mult)
        pump()
        pST0 = [banks[4], banks[7]]
        for h in range(4):
            hp, hh = divmod(h, 2)
            c.mm(pST0[hh][hh * 64:(hh + 1) * 64, hp * 128:(hp + 1) * 128], kend[0:64, h * 64:(h + 1) * 64],
                 vsb[0:64, h * 128:(h + 1) * 128])
        pCU = banks[3]
        c.mm(pCU[:, 0:8], TriIncl, at[:])
        c.mm(pCU[:, 8:16], StrictGT, at[:])
        c.mm(pCU[:, 16:24], Ones, at[:])
        pump()
        for h in range(4):
            hp, hh = divmod(h, 2)
            rows = slice(hh * 64, hh * 64 + 64)
            c.stt(S32[rows, hp, :], S32[rows, hp, :], dec[rows, hp, 0:1], pST0[hh][rows, hp * 128:(hp + 1) * 128],
                  ALU.mult, ALU.add)
        c.actv(ec[:], pCU[:, 0:24], AF.Exp)
        c.tt(Xde[:].re("p (a b) -> p a b", b=64), Xdt[:].re("p (a b) -> p a b", b=64),
             ec[:, 8:16].us(2).bc([128, 8, 64]), ALU.mult)
        pST1 = [banks[5], banks[6]]
        for h in range(4):
            hp, hh = divmod(h, 2)
            c.mm(pST1[hh][hh * 64:(hh + 1) * 64, hp * 128:(hp + 1) * 128], kend[64:128, h * 64:(h + 1) * 64],
                 vsb[64:128, h * 128:(h + 1) * 128])
        pSS = [banks[4], banks[7]]
        for g in range(2):
            c.mm(pSS[g][g * 64:(g + 1) * 64, 0:256], xtok[:, 512 + g * 64:512 + (g + 1) * 64], Xde[:, g * 256:(g + 1) * 256])
        pump()
        for h in range(4):
            hp, hh = divmod(h, 2)
            rows = slice(hh * 64, hh * 64 + 64)
            c.stt(S32[rows, hp, :], S32[rows, hp, :], dec[rows, hp, 1:2], pST1[hh][rows, hp * 128:(hp + 1) * 128],
                  ALU.mult, ALU.add)
        c.copy(Sb[0][:], S32[:], eng=c.pool)
        c.copy(dsel[0:64, :], ec[0:64, 16:20], eng=c.pool)
        c.copy(dsel[64:128, :], ec[64:128, 20:24], eng=c.pool)
        pump()
        c.tt(Ss32[:].re("p (a b) -> p a b", b=64), Ss32[:].re("p (a b) -> p a b", b=64),
             dsel[:].us(2).bc([128, 4, 64]), ALU.mult)
        for g in range(2):
            c.tt(Ss32[g * 64:(g + 1) * 64, :], Ss32[g * 64:(g + 1) * 64, :], pSS[g][g * 64:(g + 1) * 64, 0:256], ALU.add)
        c.copy(Ssb[:], Ss32[:], eng=c.pool)
        pump()

    def stage2(t, pump):
        own = t >= NPRE
        ot = t - NPRE
        par = t % 2
        xt = xb[par]
        lr1 = lr12[par]; vsb = vsb2[par]; gsil = gsil2[par]; zsil = zsil2[par]; xr = xr2[par]
        qks = qks2[par]; ktok = ktok2[par]; d1 = d12[par]
        cp(101)
        pZG = banks[3]
        c.mm(pZG[:, 0:256], lr1[0:17, :], wg2b[0:17, :])
        c.actv(Lt[:], pZG[:, 0:256], AF.Exp, scale=-1.0)
        c.actv(Lt[:], Lt[:], AF.Ln, bias=1.0)
        pump()
        pGT = banks[4]
        for hp in range(2):
            c.mm(pGT[:, hp * 128:(hp + 1) * 128], Lt[:, hp * 128:(hp + 1) * 128], TriNeg)
        c.mm(pGT[:, 256:512], StrictNeg, Lt[:, 0:256])
        c.actv(dec[:], pGT[:, 0:256].re("p (a b) -> p a b", b=128)[:, :, 63::64], AF.Exp)
        c.actv(eGe[:], pGT[:, 256:512], AF.Exp)
        c.tt(kend[:], ktok[:], eGe[:], ALU.mult)
        if own:
            c.actv(eG[:], pGT[:, 0:256], AF.Exp)
            c.actv(emG[:], pGT[:, 0:256], AF.Exp, scale=-1.0)
            c.stt(qTin[:].re("p a b -> p (a b)"), qks[:, 0:256], 0.125, eG[:], ALU.mult, ALU.mult)
            c.tt(kTin[:].re("p a b -> p (a b)"), qks[:, 256:512], emG[:], ALU.mult)
        pump()
        cp(102)
        if own:
            pAT = [banks[3], banks[5]]
            for h in range(4):
                hp, hh = divmod(h, 2)
                rows = slice(hh * 64, hh * 64 + 64)
                c.mm(pAT[hh][:, hp * 128:(hp + 1) * 128], kTin[rows, hp, :], qTin[rows, hp, :])
            for hh in range(2):
                c.tt(attm[:, hh::2, :], pAT[hh][:, 0:256].re("p (a b) -> p a b", b=128), gmask.us(1).bc([128, 2, 128]), ALU.mult)
            pump()
            pO = [banks[6], banks[7]]
            og3 = og[:].re("p (a b) -> p a b", b=128)
            for h in range(4):
                hp, hh = divmod(h, 2)
                c.mm(pO[hh][:, hp * 128:(hp + 1) * 128], attm[:, h, :], vsb[:, h * 128:(h + 1) * 128])
            for hh in range(2):
                c.actv(og3[:, hh::2, :], pO[hh][:, 0:256].re("p (a b) -> p a b", b=128), AF.Copy)
            pOb = [banks[3], banks[5]]
        cp(103)
        for cc in range(2):
            trows = slice(cc * 64, cc * 64 + 64)
            if own:
                for h in range(4):
                    hp, hh = divmod(h, 2)
                    rows = slice(hh * 64, hh * 64 + 64)
                    c.mm(pOb[hh][trows, hp * 128:(hp + 1) * 128], qTin[rows, hp, cc * 64:(cc + 1) * 64], Sb[cc][rows, hp, :])
            pST = [banks[6], banks[7]] if cc == 0 else [banks[4], banks[6]]
            for h in range(4):
                hp, hh = divmod(h, 2)
                c.mm(pST[hh][hh * 64:(hh + 1) * 64, hp * 128:(hp + 1) * 128], kend[trows, h * 64:(h + 1) * 64],
                     vsb[trows, h * 128:(h + 1) * 128])
            pump()
            for h in range(4):
                hp, hh = divmod(h, 2)
                rows = slice(hh * 64, hh * 64 + 64)
                c.stt(S32[rows, hp, :], S32[rows, hp, :], dec[rows, hp, cc:cc + 1], pST[hh][rows, hp * 128:(hp + 1) * 128],
                      ALU.mult, ALU.add)
            c.copy(Sb[1 - cc][:], S32[:], eng=c.pool)
        cp(104)
        if own:
            c.memset(gs[:], 0.0)
            for hh in range(2):
                c.tt(og3[:, hh::2, :], og3[:, hh::2, :], pOb[hh][:, 0:256].re("p (a b) -> p a b", b=128), ALU.add)
            for h in range(4):
                c.actv(junk[:, 0:128], og[:, h * 128:(h + 1) * 128], AF.Square, accum=gs[:, h:h + 1])
            c.ts(gs[:, 4:8], gs[:, 0:4], 1.0 / 128, EPS, ALU.mult, ALU.add)
            c.actv(gs[:, 4:8], gs[:, 4:8], AF.Ln)
            c.actv(gs[:, 4:8], gs[:, 4:8], AF.Exp, scale=-0.5)
            c.tt(og3, og3, gs[:, 4:8].us(2).bc([128, 4, 128]), ALU.mult)
            c.tt(og[:], og[:], gnw, ALU.mult)
            c.tt(mixed[:, 0:512], og[:], gsil[:], ALU.mult)
        cp(105)
        pCV = [banks[3], banks[5]]
        for i in range(6):
            dst = pCV[0][:, i * 128:(i + 1) * 128] if i < 4 else pCV[1][:, (i - 4) * 128:(i - 3) * 128]
            for k in range(4):
                c.mm(dst, diagW[:, i, k, :], xr[:, i, k:k + 128], start=(k == 0), stop=False)
            c.mm(dst, diagW[:, i, 4, :], ones_b[:], start=False, stop=True)
        c.actv(xc[:, 0:4, :].re("p a b -> p (a b)"), pCV[0][:], AF.Silu)
        c.actv(xc[:, 4:6, :].re("p a b -> p (a b)"), pCV[1][:, 0:256], AF.Silu)
        pump()
        pTRb = banks[0][:].bitcast(BF16)
        for i in range(5):
            c.tr(pTRb[:, i * 128:(i + 1) * 128], xc[:, i, :], ident_b[:])
        c.actv(xtok[:], pTRb[:, 0:640], AF.Copy)
        c.stt(d2[:], d1[:], -1.0, d1[:], ALU.mult, ALU.max)
        c.actv(d2[:], d2[:], AF.Exp, scale=-1.0)
        c.actv(d2[:], d2[:], AF.Ln, bias=1.0)
        c.stt(dtt[:], d1[:], 0.0, d2[:], ALU.max, ALU.add)
        if not own:
            c.ts(dtt[:], dtt[:], maskt[:, t:t + 1], None, ALU.mult)
        c.tt(at[:], dtt[:], Abc[:], ALU.mult)
        c.tt(Xdt[:].re("p (a b) -> p a b", b=64), xtok[:, 0:512].re("p (a b) -> p a b", b=64),
             dtt[:].us(2).bc([128, 8, 64]), ALU.mult)
        pCU = banks[4]
        c.mm(pCU[:, 0:8], TriIncl, at[:])
        c.mm(pCU[:, 8:16], StrictGT, at[:])
        c.mm(pCU[:, 16:24], Ones, at[:])
        c.actv(ec[:], pCU[:, 0:24], AF.Exp)
        pump()
        c.tt(Xde[:].re("p (a b) -> p a b", b=64), Xdt[:].re("p (a b) -> p a b", b=64),
             ec[:, 8:16].us(2).bc([128, 8, 64]), ALU.mult)
        if own:
            pSC = [banks[3], banks[5]]
            for g in range(2):
                rows = slice(g * 64, g * 64 + 64)
                c.mm(pSC[g][:, 0:128], xc[rows, 4, :], xc[rows, 5, :])
            for g in range(2):
                c.tt(sm[:, g, :], pSC[g][:, 0:128], TriIncl, ALU.mult)
            c.tt(Rb[:], TriIncl.us(1).bc([128, 8, 128]), at[:].us(2).bc([128, 8, 128]), ALU.mult, eng=c.pool)
            pump()
            for half in range(2):
                pSG = banks[6 + half]
                c.mm(pSG[:], StrictGT, Rb[:, half * 4:(half + 1) * 4, :].re("p a b -> p (a b)"))
                c.actv(LT[:, half * 4:(half + 1) * 4, :].re("p a b -> p (a b)"), pSG[:], AF.Exp)
                c.tt(MT[:, half * 4:(half + 1) * 4, :], LT[:, half * 4:(half + 1) * 4, :],
                     sm[:, half:half + 1, :].bc([128, 4, 128]), ALU.mult)
            pYD = banks[4]
            for h in range(8):
                c.mm(pYD[:, h * 64:(h + 1) * 64], MT[:, h, :], Xdt[:, h * 64:(h + 1) * 64])
            pYO = [banks[3], banks[5]]
            for g in range(2):
                rows = slice(g * 64, g * 64 + 64)
                c.mm(pYO[g][:, 0:256], xc[rows, 5, :], Ssb[rows, :])
            pump()
            for g in range(2):
                c.tt(yb[:, g * 256:(g + 1) * 256].re("p (a b) -> p a b", b=64), pYO[g][:, 0:256].re("p (a b) -> p a b", b=64),
                     ec[:, g * 4:(g + 1) * 4].us(2).bc([128, 4, 64]), ALU.mult)
            c.tt(yb[:], yb[:], pYD[:], ALU.add)
            c.tt(ytmp[:].re("p (a b) -> p a b", b=64), xtok[:, 0:512].re("p (a b) -> p a b", b=64),
                 dsk.us(2).bc([128, 8, 64]), ALU.mult, eng=c.pool)
            c.tt(yb[:], yb[:], ytmp[:], ALU.add)
            c.tt(yb[:], yb[:], zsil[:], ALU.mult)
            c.memset(gs[:, 0:2], 0.0)
            for g in range(2):
                c.actv(junk[:, 0:256], yb[:, g * 256:(g + 1) * 256], AF.Square, accum=gs[:, g:g + 1])
            c.ts(gs[:, 2:4], gs[:, 0:2], 1.0 / 256, EPS, ALU.mult, ALU.add)
            c.actv(gs[:, 2:4], gs[:, 2:4], AF.Ln)
            c.actv(gs[:, 2:4], gs[:, 2:4], AF.Exp, scale=-0.5)
            c.tt(yb[:].re("p (a b) -> p a b", b=256), yb[:].re("p (a b) -> p a b", b=256),
                 gs[:, 2:4].us(2).bc([128, 2, 256]), ALU.mult)
            c.tt(mixed[:, 512:1024], yb[:], snw, ALU.mult)
        cp(106)
        pSS = [banks[6], banks[7]]
        for g in range(2):
            c.mm(pSS[g][g * 64:(g + 1) * 64, 0:256], xtok[:, 512 + g * 64:512 + (g + 1) * 64], Xde[:, g * 256:(g + 1) * 256])
        pump()
        c.copy(dsel[0:64, :], ec[0:64, 16:20], eng=c.pool)
        c.copy(dsel[64:128, :], ec[64:128, 20:24], eng=c.pool)
        c.tt(Ss32[:].re("p (a b) -> p a b", b=64), Ss32[:].re("p (a b) -> p a b", b=64),
             dsel[:].us(2).bc([128, 4, 64]), ALU.mult)
        for g in range(2):
            c.tt(Ss32[g * 64:(g + 1) * 64, :], Ss32[g * 64:(g + 1) * 64, :], pSS[g][g * 64:(g + 1) * 64, 0:256], ALU.add)
        c.copy(Ssb[:], Ss32[:], eng=c.pool)
        if own:
            if stage <= 1:
                c.dma(c.sp, dbg[ot * 128:(ot + 1) * 128, :], mixed[:])
            pMTb = banks[0][:].bitcast(BF16)
            for i in range(8):
                c.tr(pMTb[:, i * 128:(i + 1) * 128], mixed[:, i * 128:(i + 1) * 128], ident_b[:])
            c.actv(mT[:].re("p a b -> p (a b)"), pMTb, AF.Copy)
            pump()
            for half in range(2):
                pOP = [banks[3], banks[5]][half]
                for fc in range(8):
                    c.mm(pOP[:], mT[:, fc, :], Wout[:, fc, half * 512:(half + 1) * 512], start=(fc == 0), stop=(fc == 7))
                c.tt(hbuf[:, half * 512:(half + 1) * 512], xt[:, half * 512:(half + 1) * 512], pOP[:], ALU.add)
            c.dma(c.sp, hs[ot * 128:(ot + 1) * 128, :], hbuf[:])
            pump()
            rmsnorm_T(hbuf[:], nfw, n2T[:, :, ot * 128:(ot + 1) * 128], (ssR, junk, xsbR))

    gen = stage1(0)
    for _ in gen:
        pass
    cp(100)
    for t in range(NT):
        nxt = stage1(t + 1) if t + 1 < NT else iter(())
        (stage2_prefix if t < NPRE else stage2)(t, lambda: next(nxt, None))
        for _ in nxt:
            pass
    while conv_jobs and stage > 1:
        issue_conv()
    c.barrier()
    esA.close()
    if stage <= 1:
        c.finish(c._last_dma_tokens())
        c.es.close()
        return nc
    build_peer(nc, c, locals())
    return nc


def build_peer(nc, c, L):
    NOWN = L["NOWN"]; TOK = L["TOK"]; banks = L["banks"]; n2T = L["n2T"]; ident_b = L["ident_b"]
    w_q = L["w_q"]; skTd = L["skTd"]; uTd = L["uTd"]; vd = L["vd"]; hs = L["hs"]; y = L["y"]; fnw = L["fnw"]
    ident_f = L["ident_f"]; iota_f = L["iota_f"]; repd = L["repd"]; ubd = L["ubd"]; vbd = L["vbd"]
    V = nc.vector
    TB = min(2, NOWN)
    BT = TB * 128
    bnc = c.dram("bnc", [TOK, 24 * 128], BF16, "Internal")
    negv0T = c.sbuf("negv0T", [128, TOK], F32)
    idx0T = c.sbuf("idx0T", [128, TOK], BF16)
    e0T = c.sbuf("e0T", [128, TOK], F32)
    reps = c.sbuf("reps", [24, 2, 128], BF16)
    c.dma(c.pool, reps[:].re("p a b -> p (a b)"), repd[:])

    es = contextlib.ExitStack()
    Wq = c.sbuf("Wq", [128, 8, 2048], BF16, es=es)
    skT = c.sbuf("skT", [128, 16, 128], BF16, es=es)
    c.dma(c.pool, Wq[:], w_q[:].re("(c p) n -> p c n", p=128))
    c.dma(c.pool, skT[:].re("p a b -> p (a b)"), skTd[:])
    qTsb = c.sbuf("qTsb", [128, 16, 128], BF16, es=es)
    S = c.sbuf("S", [128, 16, 128], F32, es=es)
    s2 = c.sbuf("s2", [128, 128], F32, es=es)
    v16 = c.sbuf("v16", [128, 16, 16], F32, es=es)
    i16 = c.sbuf("i16", [128, 8, 16], U32, es=es)
    cand = c.sbuf("cand", [128, 8, 256], F32, es=es)
    cand2 = c.sbuf("cand2", [128, 256], F32, es=es)
    b16 = c.sbuf("b16", [128, 8, 16], F32, es=es)
    eb = c.sbuf("eb", [128, 8, 16], F32, es=es)
    zz = c.sbuf("zz", [128, 24], F32, es=es)
    tk = c.sbuf("tk", [128, 3, 128], F32, es=es)
    s1p = c.sbuf("s1p", [128, 8, 128], F32, es=es)
    e1f = c.sbuf("e1f", [128, 8, 128], F32, es=es)
    bt = [c.sbuf(f"bt{i}", [128, 8, 3, 128], BF16, es=es) for i in range(2)]
    Sd = [S, c.sbuf("S_b", [128, 16, 128], F32, es=es)]

    def front(ot):
        tok = slice(ot * 128, (ot + 1) * 128)
        for hp in range(16):
            pb = banks[hp // 4]
            for dc in range(8):
                c.mm(pb[:, (hp % 4) * 128:(hp % 4 + 1) * 128], Wq[:, dc, hp * 128:(hp + 1) * 128], n2T[:, dc, tok],
                     start=(dc == 0), stop=(dc == 7))
        for q4 in range(4):
            c.actv(qTsb[:, q4 * 4:(q4 + 1) * 4, :].re("p a b -> p (a b)"), banks[q4][:], AF.Copy)
        for hp in range(16):
            c.mm(banks[4 + hp // 4][:, (hp % 4) * 128:(hp % 4 + 1) * 128], qTsb[:, hp, :], skT[:, hp, :])
        for q4 in range(4):
            c.actv(Sd[ot % 2][:, q4 * 4:(q4 + 1) * 4, :].re("p a b -> p (a b)"), banks[4 + q4][:], AF.Copy)

    front(0)
    for ot in range(NOWN):
        tok = slice(ot * 128, (ot + 1) * 128)
        S = Sd[ot % 2]
        if ot + 1 < NOWN:
            front(ot + 1)
        for hp in range(16):
            h, p = divmod(hp, 2)
            c.op(c.dve, lambda hp=hp: V.max(out=v16.ap[:, hp, 0:8], in_=S.ap[:, hp, :]), [S], [v16])
            if p == 0:
                c.op(c.dve, lambda hp=hp, h=h: V.max_index(out=i16.ap[:, h, 0:8], in_max=v16.ap[:, hp, 0:8], in_values=S.ap[:, hp, :]),
                     [S, v16], [i16])
            c.op(c.dve, lambda hp=hp: V.match_replace(out=s2.ap, in_to_replace=v16.ap[:, hp, 0:8], in_values=S.ap[:, hp, :],
                                                      imm_value=-1e30), [S, v16], [s2])
            c.op(c.dve, lambda hp=hp: V.max(out=v16.ap[:, hp, 8:16], in_=s2.ap), [s2], [v16])
            if p == 0:
                c.op(c.dve, lambda hp=hp, h=h: V.max_index(out=i16.ap[:, h, 8:16], in_max=v16.ap[:, hp, 8:16], in_values=s2.ap),
                     [s2, v16], [i16])
        v16v = v16[:].re("p (h two) r -> p h two r", two=2)
        c.tt(cand[:].re("p h (a b) -> p h a b", b=16), v16v[:, :, 0, :].us(3).bc([128, 8, 16, 16]),
             v16v[:, :, 1, :].us(2).bc([128, 8, 16, 16]), ALU.add)
        for h in range(8):
            c.op(c.dve, lambda h=h: V.max(out=b16.ap[:, h, 0:8], in_=cand.ap[:, h, :]), [cand], [b16])
            c.op(c.dve, lambda h=h: V.match_replace(out=cand2.ap, in_to_replace=b16.ap[:, h, 0:8], in_values=cand.ap[:, h, :],
                                                    imm_value=-1e30), [cand, b16], [cand2])
            c.op(c.dve, lambda h=h: V.max(out=b16.ap[:, h, 8:16], in_=cand2.ap), [cand2], [b16])
        c.tt(eb[:], b16[:], b16[:, :, 0:1].bc([128, 8, 16]), ALU.subtract)
        c.actv(eb[:], eb[:], AF.Exp)
        c.reduce(zz[:, 0:8], eb[:], ALU.add)
        c.op(c.dve, lambda: V.reciprocal(out=zz.ap[:, 8:16], in_=zz.ap[:, 0:8]), [zz], [zz])
        c.ts(zz[:, 16:24], b16[:, :, 15], -2e-4, None, ALU.add)
        tk3 = tk[:].re("p a (h r) -> p a h r", r=16)
        c.ts(tk3[:, 0], v16v[:, :, 0, :], -1.0, None, ALU.mult)
        c.copy(tk3[:, 1], i16[:])
        c.tt(tk3[:, 2], v16v[:, :, 0, :], v16v[:, :, 0, 0:1].bc([128, 8, 16]), ALU.subtract)
        c.actv(tk3[:, 2], tk3[:, 2], AF.Exp)
        c.tt(tk3[:, 2], tk3[:, 2], zz[:, 8:16].us(2).bc([128, 8, 16]), ALU.mult)
        pK = banks[ot % 2]
        for a in range(3):
            c.tr(pK[:, a * 128:(a + 1) * 128], tk[:, a, :], ident_f)
        c.actv(negv0T[:, tok], pK[:, 0:128], AF.Copy)
        c.actv(idx0T[:, tok], pK[:, 128:256], AF.Copy)
        c.actv(e0T[:, tok], pK[:, 256:384], AF.Copy)
        Sv = S[:].re("p (h two) k -> p h two k", two=2)
        B = bt[ot % 2]
        c.tt(s1p[:], Sv[:, :, 1, :], zz[:, 16:24].us(2).bc([128, 8, 128]), ALU.subtract)
        c.copy(B[:, :, 0, :], s1p[:])
        c.tt(B[:, :, 1, :], s1p[:], B[:, :, 0, :], ALU.subtract)
        c.tt(e1f[:], Sv[:, :, 1, :], v16v[:, :, 1, 0:1].bc([128, 8, 128]), ALU.subtract)
        c.actv(B[:, :, 2, :], e1f[:], AF.Exp)
        c.dma(c.sp, bnc[tok, :], B[:].re("p h a j -> p (h a j)"))
    c.barrier()
    es.close()

    es = contextlib.ExitStack()
    GT = c.sbuf("GT", [128, BT, 128], BF16, es=es)
    RG = 32
    rbuf = [c.sbuf(f"rbuf{i}", [24, RG, 128], BF16, es=es) for i in range(2)]
    e1rep = [c.sbuf(f"e1rep{i}", [128, 512], BF16, es=es) for i in range(2)]
    NR = 8
    rowb = [c.sbuf(f"rowb{i}", [128, 128], BF16, es=es) for i in range(NR)]
    oh4b = [c.sbuf(f"oh4b{i}", [128, 4, 128], BF16, es=es) for i in range(3)]
    ub = [c.sbuf(f"ub{i}", [128, 8, 512], BF16, es=es) for i in range(2)]
    vb = [c.sbuf(f"vb{i}", [128, 4, 1024], BF16, es=es) for i in range(2)]
    NH = 3
    gel = [c.sbuf(f"gel{i}", [128, BT], BF16, es=es) for i in range(NH)]
    hidT = [c.sbuf(f"hidT{i}", [128, BT], BF16, es=es) for i in range(NH)]
    hb = c.sbuf("hb", [128, 1024], F32, es=es)
    ob = c.sbuf("ob", [128, 1024], F32, es=es)
    fs = c.sbuf("fs", [128, 4], F32, es=es)
    bnc3 = bnc[:].re("t (q j) -> t q j", j=128)
    tokctr = 0
    for tb in range(NOWN // TB):
        t0 = tb * BT
        groups = [(rg, q4) for rg in range(BT // RG) for q4 in range(RG // 4)]

        def g_stage1(n):
            rg, q4 = groups[n]
            rb = rbuf[rg % 2]
            if q4 == 0:
                c.dma(c.sp, rb[:], bnc3[t0 + rg * RG:t0 + (rg + 1) * RG].re("t q j -> q t j"))
            par = n % 2
            pS = banks[0 + par]; pE = banks[2 + par]
            rhs = rb[:, q4 * 4:(q4 + 1) * 4, :].re("p a b -> p (a b)")
            c.mm(pS[:], reps[:, 0, :], rhs)
            c.mm(pE[:], reps[:, 1, :], rhs)
            lt1 = rg * RG + q4 * 4
            for u in range(4):
                gt1 = t0 + lt1 + u
                c.actv(e1rep[par][:, u * 128:(u + 1) * 128], pE[:, u * 128:(u + 1) * 128], AF.Copy, scale=e0T[:, gt1:gt1 + 1])

        def g_stage2(n):
            nonlocal tokctr
            rg, q4 = groups[n]
            lt = rg * RG + q4 * 4
            par = n % 2
            pS = banks[0 + par]; pG = banks[4 + par]
            er = e1rep[par]
            oh4 = oh4b[n % 3]
            gt0 = t0 + lt
            c.stt(oh4[:], iota_f.us(1).bc([128, 4, 128]), 0.0, idx0T[:, gt0:gt0 + 4].us(2).bc([128, 4, 128]),
                  ALU.add, ALU.is_equal)
            for u in range(4):
                gt = gt0 + u
                rw = rowb[tokctr % NR]
                tokctr += 1
                c.stt(rw[:], pS[:, u * 128:(u + 1) * 128], negv0T[:, gt:gt + 1], er[:, u * 128:(u + 1) * 128],
                      ALU.is_ge, ALU.mult)
                c.mm(pG[:, u * 128:(u + 1) * 128], rw[:], oh4[:, u, :])
            c.actv(GT[:, lt:lt + 4, :].re("p a b -> p (a b)"), pG[:], AF.Copy)

        g_stage1(0)
        for n in range(len(groups)):
            if n + 1 < len(groups):
                g_stage1(n + 1)
            g_stage2(n)
        pOut = [[banks[4], banks[5]], [banks[6], banks[7]]]
        chunks = [(g, ci) for g in range(NG) for ci in range(4)]

        def emit_act(n):
            g, ci = chunks[n]
            k = g % 2
            if ci == 0:
                c.dma(c.sp, ub[k][:], ubd[g][:].re("(c p) e -> p c e", p=128))
                c.dma(c.sp, vb[k][:], vbd[g][:].re("(c p) d -> p c d", p=128))
            pA = banks[n % 2]
            for dc in range(8):
                c.mm(pA[:, 0:BT], ub[k][:, dc, ci * 128:(ci + 1) * 128], n2T[:, dc, t0:t0 + BT], start=(dc == 0), stop=(dc == 7))
            c.actv(gel[n % NH][:], pA[:, 0:BT], AF.Gelu)
            c.tt(hidT[n % NH][:], gel[n % NH][:], GT[:, :, g * 4 + ci], ALU.mult)

        def emit_out(n):
            g, ci = chunks[n]
            k = g % 2
            for tt in range(TB):
                for half in range(2):
                    c.mm(pOut[tt][half][:], hidT[n % NH][:, tt * 128:(tt + 1) * 128], vb[k][:, ci, half * 512:(half + 1) * 512],
                         start=(n == 0), stop=(n == len(chunks) - 1))

        emit_act(0)
        for n in range(len(chunks)):
            if n + 1 < len(chunks):
                emit_act(n + 1)
            emit_out(n)
        for tl in range(TB):
            ot = tb * TB + tl
            c.dma(c.sp, hb[:], hs[ot * 128:(ot + 1) * 128, :])
            for half in range(2):
                c.tt(hb[:, half * 512:(half + 1) * 512], hb[:, half * 512:(half + 1) * 512], pOut[tl][half][:], ALU.add)
            c.memset(fs[:, 0:1], 0.0)
            c.actv(ob[:], hb[:], AF.Square, accum=fs[:, 0:1])
            c.ts(fs[:, 1:2], fs[:, 0:1], 1.0 / D, EPS, ALU.mult, ALU.add)
            c.actv(fs[:, 2:3], fs[:, 1:2], AF.Ln)
            c.actv(fs[:, 3:4], fs[:, 2:3], AF.Exp, scale=-0.5)
            c.stt(ob[:], hb[:], fs[:, 3:4], fnw, ALU.mult, ALU.mult)
            c.dma(c.sp, y[ot * 128:(ot + 1) * 128, :], ob[:])
    c.barrier()
    c.finish(c._last_dma_tokens())
    es.close()
    c.es.close()

def make_consts():
    s = np.arange(128)[:, None]; t = np.arange(128)[None, :]
    same = (s // 64) == (t // 64)
    ident = (s == t).astype(np.float32)
    TriNeg = np.where((s <= t) & same, -1.0 / 16, 0.0).astype(np.float32)
    StrictNeg = np.where((s > t) & same, -1.0 / 16, 0.0).astype(np.float32)
    gmask = ((t >= s) & same).astype(np.float32)
    TriIncl = (s <= t).astype(np.float32)
    StrictGT = (s > t).astype(np.float32)
    Ones = np.ones((128, 128), np.float32)
    iota = np.broadcast_to(t.astype(np.float32), (128, 128))
    return np.ascontiguousarray(np.concatenate([ident, TriNeg, StrictNeg, gmask, TriIncl, StrictGT, Ones, iota], axis=1))


def prep_shared(inp):
    f = lambda a: np.ascontiguousarray(np.asarray(a, dtype=np.float32))
    pp = np.zeros((128, 46), np.float32)
    pp[:, 0:8] = f(inp["norm_mix_w"])[0].reshape(8, 128).T
    pp[:, 8:16] = f(inp["norm_ffn_w"])[0].reshape(8, 128).T
    cw = f(inp["ssd_conv_w"])[0]
    pp[:, 16:40] = cw.reshape(4, 6, 128).transpose(2, 1, 0).reshape(128, 24)
    pp[:, 40:46] = f(inp["ssd_conv_b"])[0].reshape(6, 128).T
    bc = np.zeros((128, 24 + 512 + 512 + 1024), np.float32)
    bc[:, 0:8] = f(inp["ssd_dt_bias"])[0][None]
    bc[:, 8:16] = f(inp["ssd_a_log"])[0][None]
    bc[:, 16:24] = f(inp["ssd_d"])[0][None]
    bc[:, 24:536] = np.tile(f(inp["gla_norm_w"])[0], 4)[None]
    bc[:, 536:1048] = f(inp["ssd_norm_w"])[0][None]
    bc[:, 1048:2072] = f(inp["norm_final_w"])[None]
    wg2b = np.concatenate([f(inp["gla_w_gate2"])[0], f(inp["gla_b_gate"])[0][None]], axis=0)
    sk = f(inp["peer_sub_keys"])[0]
    skT = np.ascontiguousarray(sk.transpose(3, 0, 1, 2).reshape(128, 16 * 128))
    rep = np.zeros((24, 2, 128), np.float32)
    for q in range(24):
        hh, a = divmod(q, 3)
        rep[q, 0 if a < 2 else 1, hh * 16:(hh + 1) * 16] = 1.0
    return {
        "repd": np.ascontiguousarray(rep.reshape(24, 256)),
        "constd": make_consts(), "ppd": pp, "bcd": bc, "wg2bd": np.ascontiguousarray(wg2b),
        "w_in": f(inp["w_in"])[0], "w_out": f(inp["w_out"])[0], "w_q": f(inp["peer_w_query"])[0],
        "skTd": skT, "uTd": np.ascontiguousarray(f(inp["peer_u"])[0].T), "vd": f(inp["peer_v"])[0],
    }


def kernel(**inputs):
    x = np.asarray(inputs["x"], dtype=np.float32)
    sh = prep_shared(inputs)
    NPRE, NOWN = 48, 16
    in_maps = []
    for core in range(8):
        b, j = divmod(core, 4)
        xin = np.zeros((64 * 128, 1024), np.float32)
        mask = np.zeros((128, 64), np.float32)
        lo = (j - 3) * 2048
        hi = (j + 1) * 2048
        src_lo = max(lo, 0)
        n = hi - src_lo
        xin[64 * 128 - n:] = x[b, src_lo:hi]
        mask[:, 64 - n // 128:] = 1.0
        m = dict(sh)
        m["xin"] = xin
        m["maskd"] = mask
        in_maps.append(m)
    nc = build_program(NPRE, NOWN, stage=99)
    res = run_bass_kernel_spmd(nc, in_maps, core_ids=list(range(8)))
    out = np.zeros((2, 8192, 1024), np.float32)
    for core in range(8):
        b, j = divmod(core, 4)
        out[b, j * 2048:(j + 1) * 2048] = res.results[core]["y"]
    return out
```

```python
import contextlib
import numpy as np
import concourse.bass as bass
import concourse.mybir as mybir
from concourse.bass_utils import run_bass_kernel_spmd

F32 = mybir.dt.float32
BF16 = mybir.dt.bfloat16
U32 = mybir.dt.uint32
I32 = mybir.dt.int32
AF = mybir.ActivationFunctionType
ALU = mybir.AluOpType
AX = mybir.AxisListType

SEM_CHUNK = 4096
STRICT_SAME_ENGINE = False


class Buf:
    def __init__(self, ap, name=""):
        self.ap = ap
        self.name = name
        self.last_write = None
        self.readers = []
        self.is_psum = False

    def __getitem__(self, key):
        return View(self, self.ap[key])

    def v(self, ap):
        return View(self, ap)

    def sub(self, key, name=""):
        return Buf(self.ap[key], name or self.name)


class View:
    def __init__(self, buf, ap):
        self.buf = buf
        self.ap = ap

    def __getitem__(self, key):
        return View(self.buf, self.ap[key])

    def re(self, s, **kw):
        return View(self.buf, self.ap.rearrange(s, **kw))

    def bc(self, shape):
        return View(self.buf, self.ap.broadcast_to(shape))

    def us(self, axis):
        return View(self.buf, self.ap.unsqueeze(axis))

    def bitcast(self, dt):
        return View(self.buf, self.ap.bitcast(dt))


class EngState:
    def __init__(self, ctx, name, handle):
        self.ctx = ctx
        self.name = name
        self.h = handle
        self.count = 0
        self.sems = []
        self.seen = {}

    def sem_for(self, n):
        idx = (n - 1) // SEM_CHUNK
        while len(self.sems) <= idx:
            self.sems.append(self.ctx.new_sem(f"{self.name}{len(self.sems)}"))
        return self.sems[idx], (n - 1) % SEM_CHUNK + 1


class Ctx:
    def __init__(self, nc):
        self.nc = nc
        self.es = contextlib.ExitStack()
        self.nsem = 0
        self.pe = EngState(self, "pe", nc.tensor)
        self.act = EngState(self, "act", nc.scalar)
        self.dve = EngState(self, "dve", nc.vector)
        self.pool = EngState(self, "pool", nc.gpsimd)
        self.sp = EngState(self, "sp", nc.sync)
        self.engs = [self.pe, self.act, self.dve, self.pool, self.sp]
        self.dma_rings = {}
        self.all_dma_tokens = []

    def new_sem(self, name):
        self.nsem += 1
        return self.es.enter_context(self.nc.semaphore(f"s_{name}_{self.nsem}"))

    def sbuf(self, name, shape, dt, es=None):
        t = (es or self.es).enter_context(self.nc.sbuf_tensor(name, list(shape), dt))
        return Buf(t[:] if hasattr(t, "__getitem__") else t, name)

    def psum(self, name, shape, dt, es=None):
        t = (es or self.es).enter_context(self.nc.psum_tensor(name, list(shape), dt))
        b = Buf(t[:], name)
        b.is_psum = True
        return b

    def dram(self, name, shape, dt, kind):
        t = self.nc.dram_tensor(name, list(shape), dt, kind=kind)
        return Buf(t.ap(), name)

    def _wait(self, eng, tok):
        if tok is None:
            return
        if tok[0] == "e":
            _, src, n = tok
            if src is eng:
                return
            if eng.seen.get(src.name, 0) >= n:
                return
            sem, val = src.sem_for(n)
            eng.h.wait_ge(sem, val)
            eng.seen[src.name] = n
        else:
            _, sid, sem, val = tok
            key = ("d", sid)
            if eng.seen.get(key, 0) >= val:
                return
            eng.h.wait_ge(sem, val)
            eng.seen[key] = val

    def _deps(self, eng, reads, writes, same_engine_raw=True):
        toks = []
        for b in reads:
            if b.last_write is not None:
                toks.append(b.last_write)
            if b.is_psum:
                toks.extend(b.readers)
        for b in writes:
            if b.last_write is not None:
                toks.append(b.last_write)
            toks.extend(b.readers)
        for tok in toks:
            if tok[0] == "e" and tok[1] is eng:
                continue
            self._wait(eng, tok)
        if eng is not self.pe and same_engine_raw:
            mx = 0
            for b in reads:
                lw = b.last_write
                if lw is not None and lw[0] == "e" and lw[1] is eng:
                    mx = max(mx, lw[2])
            if STRICT_SAME_ENGINE:
                for b in writes:
                    lw = b.last_write
                    if lw is not None and lw[0] == "e" and lw[1] is eng:
                        mx = max(mx, lw[2])
                    for r in b.readers:
                        if r[0] == "e" and r[1] is eng:
                            mx = max(mx, r[2])
            if mx > eng.seen.get(eng.name, 0):
                sem, val = eng.sem_for(mx)
                eng.h.wait_ge(sem, val)
                eng.seen[eng.name] = mx

    def _commit(self, tok, reads, writes):
        for b in writes:
            b.last_write = tok
            b.readers = []
        for b in reads:
            if b not in writes:
                b.readers.append(tok)
                if len(b.readers) > 64:
                    b.readers = self._prune(b.readers)

    @staticmethod
    def _prune(readers):
        best = {}
        out = []
        for t in readers:
            if t[0] == "e":
                k = t[1].name
                if k not in best or best[k][2] < t[2]:
                    best[k] = t
            else:
                out.append(t)
        return out + list(best.values())

    def op(self, eng, fn, reads, writes):
        reads = [r.buf if isinstance(r, View) else r for r in reads]
        writes = [w.buf if isinstance(w, View) else w for w in writes]
        self._deps(eng, reads, writes)
        ins = fn()
        eng.count += 1
        sem, val = eng.sem_for(eng.count)
        ins.then_inc(sem, 1)
        self._commit(("e", eng, eng.count), reads, writes)
        return ins

    def dma(self, eng, out, in_, ring=8, **kw):
        rb, wb = in_.buf, out.buf
        self._deps(eng, [rb], [wb], same_engine_raw=False)
        key = eng.name
        if key not in self.dma_rings:
            self.dma_rings[key] = {"n": 0, "sems": [], "last": {}}
        R = self.dma_rings[key]
        slot = R["n"] % ring
        while len(R["sems"]) <= slot:
            R["sems"].append(self.new_sem(f"dma_{key}{len(R['sems'])}"))
        sem = R["sems"][slot]
        val = 16 * (R["n"] // ring + 1)
        sid = (key, slot)
        if val > 16:
            self._wait(eng, ("d", sid, sem, val - 16))
        R["n"] += 1
        ins = eng.h.dma_start(out=out.ap, in_=in_.ap, **kw)
        ins.then_inc(sem, 16)
        tok = ("d", sid, sem, val)
        self._commit(tok, [rb], [wb])
        self.all_dma_tokens.append(tok)
        return tok

    def barrier(self):
        for e in self.engs:
            for s in self.engs:
                if s is not e and s.count > 0:
                    self._wait(e, ("e", s, s.count))
            for tok in self._last_dma_tokens():
                self._wait(e, tok)

    def _last_dma_tokens(self):
        last = {}
        for tok in self.all_dma_tokens:
            last[tok[1]] = tok
        self.all_dma_tokens = list(last.values())
        return self.all_dma_tokens

    def finish(self, toks):
        for t in toks:
            self._wait(self.sp, t)

    def mm(self, out, lhsT, rhs, start=True, stop=True, **kw):
        return self.op(self.pe, lambda: self.nc.tensor.matmul(out.ap, lhsT.ap, rhs.ap, start=start, stop=stop, **kw),
                       [lhsT, rhs] + ([] if start else []), [out])

    def tr(self, out, in_, ident):
        return self.op(self.pe, lambda: self.nc.tensor.transpose(out.ap, in_.ap, ident.ap), [in_, ident], [out])

    def actv(self, out, in_, func, bias=None, scale=None, accum=None, eng=None):
        reads = [in_]
        kw = {}
        if bias is not None:
            if isinstance(bias, View):
                reads.append(bias); kw["bias"] = bias.ap
            else:
                kw["bias"] = bias
        if scale is not None:
            if isinstance(scale, View):
                reads.append(scale); kw["scale"] = scale.ap
            else:
                kw["scale"] = scale
        writes = [out]
        if accum is not None:
            writes.append(accum); kw["accum_out"] = accum.ap
        return self.op(self.act, lambda: self.nc.scalar.activation(out.ap, in_.ap, func, **kw), reads, writes)

    def _ve(self, eng):
        return eng or self.dve

    def tt(self, out, a, b, op, eng=None):
        e = self._ve(eng)
        return self.op(e, lambda: e.h.tensor_tensor(out.ap, a.ap, b.ap, op), [a, b], [out])

    def ts(self, out, a, s1, s2, op0, op1=None, accum=None, eng=None):
        e = self._ve(eng)
        reads = [a]
        v1 = s1.ap if isinstance(s1, View) else s1
        v2 = s2.ap if isinstance(s2, View) else s2
        if isinstance(s1, View): reads.append(s1)
        if isinstance(s2, View): reads.append(s2)
        writes = [out]
        kw = {}
        if op1 is not None: kw["op1"] = op1
        if accum is not None:
            kw["accum_out"] = accum.ap; writes.append(accum)
        return self.op(e, lambda: e.h.tensor_scalar(out.ap, a.ap, v1, v2, op0, **kw), reads, writes)

    def stt(self, out, a, s, b, op0, op1, accum=None, eng=None):
        e = self._ve(eng)
        reads = [a, b]
        sv = s.ap if isinstance(s, View) else s
        if isinstance(s, View): reads.append(s)
        writes = [out]
        kw = {}
        if accum is not None:
            kw["accum_out"] = accum.ap; writes.append(accum)
        return self.op(e, lambda: e.h.scalar_tensor_tensor(out.ap, a.ap, sv, b.ap, op0, op1, **kw), reads, writes)

    def copy(self, out, in_, eng=None):
        e = self._ve(eng)
        return self.op(e, lambda: e.h.tensor_copy(out.ap, in_.ap), [in_], [out])

    def memset(self, out, val, eng=None):
        e = self._ve(eng)
        return self.op(e, lambda: e.h.memset(out.ap, val), [], [out])

    def reduce(self, out, in_, op, axis=AX.X, eng=None):
        e = self._ve(eng)
        return self.op(e, lambda: e.h.tensor_reduce(out.ap, in_.ap, axis, op), [in_], [out])

EPS = 1e-6
D = 1024
IN_TOTAL = 2840
C_Q, C_K, C_V, C_LR, C_GO, C_Z, C_XBC, C_DT = 0, 256, 512, 1024, 1040, 1552, 2064, 2832
NEXP_GROUP = 512
NG = 16384 // NEXP_GROUP


class _Stop(Exception):
    pass


def build_program(NPRE, NOWN, stage=99, stop=None):
    def cp(k):
        if stop is not None and k == stop:
            raise _Stop()
    try:
        return _build_program(NPRE, NOWN, stage, cp)
    except _Stop as e:
        c = _LAST[0]
        c.barrier(); c.finish(c._last_dma_tokens())
        return c.nc


_LAST = [None]


def _build_program(NPRE, NOWN, stage, cp):
    nc = bass.Bass("TRN2", target_bir_lowering=False)
    c = Ctx(nc)
    _LAST[0] = c
    NT = NPRE + NOWN
    TOK = NOWN * 128
    xin = c.dram("xin", [NT * 128, D], F32, "ExternalInput")
    maskd = c.dram("maskd", [128, NT], F32, "ExternalInput")
    constd = c.dram("constd", [128, 8 * 128], F32, "ExternalInput")
    ppd = c.dram("ppd", [128, 46], F32, "ExternalInput")
    bcd = c.dram("bcd", [128, 24 + 512 + 512 + 1024], F32, "ExternalInput")
    wg2bd = c.dram("wg2bd", [17, 256], F32, "ExternalInput")
    w_in = c.dram("w_in", [D, IN_TOTAL], F32, "ExternalInput")
    w_out = c.dram("w_out", [D, D], F32, "ExternalInput")
    w_q = c.dram("w_q", [D, 2048], F32, "ExternalInput")
    skTd = c.dram("skTd", [128, 16 * 128], F32, "ExternalInput")
    uTd = c.dram("uTd", [D, 16384], F32, "ExternalInput")
    vd = c.dram("vd", [16384, D], F32, "ExternalInput")
    repd = c.dram("repd", [24, 256], F32, "ExternalInput")
    y = c.dram("y", [TOK, D], F32, "ExternalOutput")
    hs = c.dram("hs", [TOK, D], F32, "Internal" if stage > 1 else "ExternalOutput")
    if stage <= 1:
        dbg = c.dram("dbg", [TOK, D], BF16, "ExternalOutput")
    if stage > 2:
        Gd = c.dram("Gd", [NG * 128, TOK * 4], BF16, "Internal")

    ubd_t = nc.dram_tensor("ubd", [D, 16384], BF16, kind="Internal").ap()
    vbd_t = nc.dram_tensor("vbd", [16384, D], BF16, kind="Internal").ap()
    ubd = [Buf(ubd_t[:, g * 512:(g + 1) * 512], f"ubd{g}") for g in range(NG)]
    vbd = [Buf(vbd_t[g * 512:(g + 1) * 512, :], f"vbd{g}") for g in range(NG)]
    conv_jobs = [("u", g) for g in range(NG)] + [("v", g) for g in range(NG)]

    def issue_conv():
        if not conv_jobs or stage <= 1:
            return
        kind, g = conv_jobs.pop(0)
        if kind == "u":
            c.dma(c.pool, ubd[g][:], uTd[:, g * 512:(g + 1) * 512])
        else:
            c.dma(c.pool, vbd[g][:], vd[g * 512:(g + 1) * 512, :])

    const = c.sbuf("const", [128, 8, 128], F32)
    ident_f = const[:, 0, :]; TriNeg = const[:, 1, :]; StrictNeg = const[:, 2, :]; gmask = const[:, 3, :]
    TriIncl = const[:, 4, :]; StrictGT = const[:, 5, :]; Ones = const[:, 6, :]; iota_f = const[:, 7, :]
    ident_b = c.sbuf("ident_b", [128, 128], BF16)
    pp = c.sbuf("pp", [128, 46], F32)
    bcs = c.sbuf("bcs", [128, 24 + 512 + 512 + 1024], F32)
    maskt = c.sbuf("maskt", [128, NT], F32)
    Abc = c.sbuf("Abc", [128, 8], F32)
    n2T = c.sbuf("n2T", [128, 8, TOK], BF16)
    banks = [c.psum(f"bank{i}", [128, 512], F32) for i in range(8)]
    bctr = [0]

    def nb():
        b = banks[bctr[0] % 8]
        bctr[0] += 1
        return b

    c.dma(c.sp, const[:].re("p a b -> p (a b)"), constd[:])
    c.dma(c.sp, pp[:], ppd[:])
    c.dma(c.sp, bcs[:], bcd[:])
    c.dma(c.sp, maskt[:], maskd[:])
    c.copy(ident_b[:], ident_f)
    c.actv(Abc[:], bcs[:, 8:16], AF.Exp)
    c.ts(Abc[:], Abc[:], -1.0, None, ALU.mult)
    nmw = pp[:, 0:8]; nfw = pp[:, 8:16]; cw = pp[:, 16:40].re("p (i k) -> p i k", k=4); cb = pp[:, 40:46]
    dtb = bcs[:, 0:8]; dsk = bcs[:, 16:24]; gnw = bcs[:, 24:536]; snw = bcs[:, 536:1048]; fnw = bcs[:, 1048:2072]

    def rmsnorm_T(xt, wcol, outT, scr):
        ss, junk, xsb = scr
        c.memset(ss[:, 0:1], 0.0)
        c.actv(junk[:], xt, AF.Square, accum=ss[:, 0:1])
        c.ts(ss[:, 1:2], ss[:, 0:1], 1.0 / D, EPS, ALU.mult, ALU.add)
        c.actv(ss[:, 2:3], ss[:, 1:2], AF.Ln)
        c.actv(ss[:, 3:4], ss[:, 2:3], AF.Exp, scale=-0.5)
        c.ts(xsb[:], xt, ss[:, 3:4], None, ALU.mult)
        pb = banks[0]
        pbb = pb[:].bitcast(BF16)
        for dc in range(8):
            c.tr(pbb[:, dc * 128:(dc + 1) * 128], xsb[:, dc * 128:(dc + 1) * 128], ident_b[:])
        c.tt(outT, pbb.re("p (a b) -> p a b", b=128), wcol.us(2).bc([128, 8, 128]), ALU.mult)

    esA = contextlib.ExitStack()
    Win = c.sbuf("Win", [128, 8, IN_TOTAL], BF16, es=esA)
    Wout = c.sbuf("Wout", [128, 8, D], BF16, es=esA)
    wg2b = c.sbuf("wg2b", [17, 256], F32, es=esA)
    for dc in range(8):
        c.dma(c.pool, Win[:, dc, :], w_in[dc * 128:(dc + 1) * 128, :])
    c.dma(c.pool, Wout[:], w_out[:].re("(c p) n -> p c n", p=128))
    c.dma(c.sp, wg2b[:], wg2bd[:])
    xb = [c.sbuf(f"xb{i}", [128, D], F32, es=esA) for i in range(2)]
    ss = c.sbuf("ss", [128, 4], F32, es=esA)
    junk = c.sbuf("junk", [128, D], F32, es=esA)
    xsb = c.sbuf("xsb", [128, D], BF16, es=esA)
    nT = c.sbuf("nT", [128, 8, 128], BF16, es=esA)
    lr1 = c.sbuf("lr1", [17, 128], F32, es=esA)
    Lt = c.sbuf("Lt", [128, 256], F32, es=esA)
    eG = c.sbuf("eG", [128, 256], F32, es=esA)
    emG = c.sbuf("emG", [128, 256], F32, es=esA)
    eGe = c.sbuf("eGe", [128, 256], F32, es=esA)
    dec = c.sbuf("dec", [128, 2, 2], F32, es=esA)
    qTin = c.sbuf("qTin", [128, 2, 128], BF16, es=esA)
    kTin = c.sbuf("kTin", [128, 2, 128], BF16, es=esA)
    kend = c.sbuf("kend", [128, 256], BF16, es=esA)
    vsb = c.sbuf("vsb", [128, 512], BF16, es=esA)
    gsil = c.sbuf("gsil", [128, 512], BF16, es=esA)
    zsil = c.sbuf("zsil", [128, 512], BF16, es=esA)
    attm = c.sbuf("attm", [128, 4, 128], BF16, es=esA)
    S32 = c.sbuf("S32", [128, 2, 128], F32, es=esA)
    Sb = [c.sbuf(f"Sb{i}", [128, 2, 128], BF16, es=esA) for i in range(2)]
    gs = c.sbuf("gs", [128, 8], F32, es=esA)
    og = c.sbuf("og", [128, 512], F32, es=esA)
    mixed = c.sbuf("mixed", [128, 1024], BF16, es=esA)
    xr = c.sbuf("xr", [128, 6, 131], BF16, es=esA)
    cv = c.sbuf("cv", [128, 6, 128], F32, es=esA)
    cvt = c.sbuf("cvt", [128, 6, 128], F32, es=esA)
    xc = c.sbuf("xc", [128, 6, 128], BF16, es=esA)
    xtok = c.sbuf("xtok", [128, 640], BF16, es=esA)
    d1 = c.sbuf("d1", [128, 8], F32, es=esA)
    d2 = c.sbuf("d2", [128, 8], F32, es=esA)
    dtt = c.sbuf("dtt", [128, 8], F32, es=esA)
    at = c.sbuf("at", [128, 8], F32, es=esA)
    ec = c.sbuf("ec", [128, 24], F32, es=esA)
    Xdt = c.sbuf("Xdt", [128, 512], BF16, es=esA)
    Xde = c.sbuf("Xde", [128, 512], BF16, es=esA)
    sm = c.sbuf("sm", [128, 2, 128], F32, es=esA)
    Rb = c.sbuf("Rb", [128, 8, 128], F32, es=esA)
    LT = c.sbuf("LT", [128, 8, 128], F32, es=esA)
    MT = c.sbuf("MT", [128, 8, 128], BF16, es=esA)
    yb = c.sbuf("yb", [128, 512], F32, es=esA)
    ytmp = c.sbuf("ytmp", [128, 512], F32, es=esA)
    Ss32 = c.sbuf("Ss32", [128, 256], F32, es=esA)
    Ssb = c.sbuf("Ssb", [128, 256], BF16, es=esA)
    dsel = c.sbuf("dsel", [128, 4], F32, es=esA)
    mT = c.sbuf("mT", [128, 8, 128], BF16, es=esA)
    hbuf = c.sbuf("hbuf", [128, D], F32, es=esA)

    c.memset(lr1[:], 1.0)
    c.memset(S32[:], 0.0)
    c.memset(Sb[0][:], 0.0)
    c.memset(Ss32[:], 0.0)
    c.memset(Ssb[:], 0.0)
    c.memset(xr[:], 0.0)

    def fm(outv, col0, M):
        for dc in range(8):
            c.mm(outv, Win[:, dc, col0:col0 + M], nT[:, dc, :], start=(dc == 0), stop=(dc == 7))

    def tm(outv, col0, N):
        for dc in range(8):
            c.mm(outv, nT[:, dc, :], Win[:, dc, col0:col0 + N], start=(dc == 0), stop=(dc == 7))

    xsb2 = [xsb, c.sbuf("xsbB", [128, D], BF16, es=esA)]
    ss2 = [ss, c.sbuf("ssB", [128, 4], F32, es=esA)]
    nT2 = [nT, c.sbuf("nTB", [128, 8, 128], BF16, es=esA)]
    lr12 = [lr1, c.sbuf("lr1B", [17, 128], F32, es=esA)]
    vsb2 = [vsb, c.sbuf("vsbB", [128, 512], BF16, es=esA)]
    gsil2 = [gsil, c.sbuf("gsilB", [128, 512], BF16, es=esA)]
    zsil2 = [zsil, c.sbuf("zsilB", [128, 512], BF16, es=esA)]
    xr2 = [xr, c.sbuf("xrB", [128, 6, 131], BF16, es=esA)]
    qks2 = [c.sbuf(f"qks{i}", [128, 512], F32, es=esA) for i in range(2)]
    ktok2 = [c.sbuf(f"ktok{i}", [128, 256], F32, es=esA) for i in range(2)]
    d12 = [d1, c.sbuf("d1B", [128, 8], F32, es=esA)]
    ssR = c.sbuf("ssR", [128, 4], F32, es=esA)
    xsbR = c.sbuf("xsbR", [128, D], BF16, es=esA)
    c.memset(lr12[1][:], 1.0)
    diagW = c.sbuf("diagW", [128, 6, 5, 128], BF16, es=esA)
    ones_b = c.sbuf("ones_b", [128, 128], BF16, es=esA)
    c.memset(ones_b[:], 1.0)
    for i in range(6):
        for k in range(4):
            c.ts(diagW[:, i, k, :], ident_f, cw[:, i, k:k + 1], None, ALU.mult)
        c.ts(diagW[:, i, 4, :], ident_f, cb[:, i:i + 1], None, ALU.mult)
    c.memset(xr2[1][:], 0.0)

    def stage1(t):
        own = t >= NPRE
        par = t % 2
        xt = xb[par]
        nTp = nT2[par]
        c.dma(c.sp, xt[:], xin[t * 128:(t + 1) * 128, :])
        issue_conv()
        sst, xs_ = ss2[par], xsb2[par]
        c.memset(sst[:, 0:1], 0.0)
        c.actv(junk[:], xt[:], AF.Square, accum=sst[:, 0:1])
        c.ts(sst[:, 1:2], sst[:, 0:1], 1.0 / D, EPS, ALU.mult, ALU.add)
        c.actv(sst[:, 2:3], sst[:, 1:2], AF.Ln)
        c.actv(sst[:, 3:4], sst[:, 2:3], AF.Exp, scale=-0.5)
        c.actv(xs_[:], xt[:], AF.Copy, scale=sst[:, 3:4])
        cp(80) if t == 0 else None
        yield
        pbb = banks[0][:].bitcast(BF16)
        for dc in range(8):
            c.tr(pbb[:, dc * 128:(dc + 1) * 128], xs_[:, dc * 128:(dc + 1) * 128], ident_b[:])
        c.tt(nTp[:], pbb.re("p (a b) -> p a b", b=128), nmw.us(2).bc([128, 8, 128]), ALU.mult)
        cp(81) if t == 0 else None
        yield

        def fm(outv, col0, M):
            for dc in range(8):
                c.mm(outv, Win[:, dc, col0:col0 + M], nTp[:, dc, :], start=(dc == 0), stop=(dc == 7))

        def tm(outv, col0, N):
            for dc in range(8):
                c.mm(outv, nTp[:, dc, :], Win[:, dc, col0:col0 + N], start=(dc == 0), stop=(dc == 7))

        xrp = xr2[par]
        c.copy(xrp[:, :, 0:3], xr2[1 - par][:, :, 128:131], eng=c.pool)
        pb = banks[1]
        for i in range(4):
            fm(pb[:, i * 128:(i + 1) * 128], C_XBC + i * 128, 128)
        c.actv(xrp[:, 0:4, 3:131], pb[:].re("p (a b) -> p a b", b=128), AF.Copy)
        cp(82) if t == 0 else None
        yield
        pb = banks[2]
        for i in range(2):
            fm(pb[:, i * 128:(i + 1) * 128], C_XBC + 512 + i * 128, 128)
        fm(pb[0:16, 256:384], C_LR, 16)
        c.actv(xrp[:, 4:6, 3:131], pb[:, 0:256].re("p (a b) -> p a b", b=128), AF.Copy)
        c.actv(lr12[par][0:16, :], pb[0:16, 256:384], AF.Copy)
        cp(83) if t == 0 else None
        yield
        pb = banks[1]
        tm(pb[:, 0:512], C_V, 512)
        c.actv(vsb2[par][:], pb[:, 0:512], AF.Copy)
        cp(84) if t == 0 else None
        yield
        pb = banks[2]
        tm(pb[:, 0:256], C_K, 256)
        tm(pb[:, 256:264], C_DT, 8)
        c.copy(ktok2[par][:], pb[:, 0:256])
        c.tt(d12[par][:], pb[:, 256:264], dtb, ALU.add)
        cp(85) if t == 0 else None
        yield
        if own:
            pb = banks[1]
            fm(pb[:, 0:128], C_Q, 128)
            fm(pb[:, 128:256], C_Q + 128, 128)
            fm(pb[:, 256:384], C_K, 128)
            fm(pb[:, 384:512], C_K + 128, 128)
            c.copy(qks2[par][:], pb[:])
            cp(86) if t == 0 else None
            yield
            pb = banks[2]
            tm(pb[:, 0:512], C_GO, 512)
            c.actv(gsil2[par][:], pb[:], AF.Silu)
            cp(87) if t == 0 else None
            yield
            pb = banks[1]
            tm(pb[:, 0:512], C_Z, 512)
            c.actv(zsil2[par][:], pb[:], AF.Silu)
            cp(88) if t == 0 else None
            yield

    def stage2_prefix(t, pump):
        par = t % 2
        lr1 = lr12[par]; vsb = vsb2[par]; xr = xr2[par]; ktok = ktok2[par]; d1 = d12[par]
        pCV = [banks[3], banks[5]]
        for i in range(6):
            dst = pCV[0][:, i * 128:(i + 1) * 128] if i < 4 else pCV[1][:, (i - 4) * 128:(i - 3) * 128]
            for k in range(4):
                c.mm(dst, diagW[:, i, k, :], xr[:, i, k:k + 128], start=(k == 0), stop=False)
            c.mm(dst, diagW[:, i, 4, :], ones_b[:], start=False, stop=True)
        pZG = banks[4]
        c.mm(pZG[:, 0:256], lr1[0:17, :], wg2b[0:17, :])
        c.actv(xc[:, 0:4, :].re("p a b -> p (a b)"), pCV[0][:], AF.Silu)
        c.actv(xc[:, 4:6, :].re("p a b -> p (a b)"), pCV[1][:, 0:256], AF.Silu)
        c.actv(Lt[:], pZG[:, 0:256], AF.Exp, scale=-1.0)
        c.actv(Lt[:], Lt[:], AF.Ln, bias=1.0)
        c.stt(d2[:], d1[:], -1.0, d1[:], ALU.mult, ALU.max)
        pump()
        pTRb = banks[0][:].bitcast(BF16)
        for i in range(5):
            c.tr(pTRb[:, i * 128:(i + 1) * 128], xc[:, i, :], ident_b[:])
        pGT = banks[6]
        for hp in range(2):
            c.mm(pGT[:, hp * 128:(hp + 1) * 128], Lt[:, hp * 128:(hp + 1) * 128], TriNeg)
        c.mm(pGT[:, 256:512], StrictNeg, Lt[:, 0:256])
        c.actv(d2[:], d2[:], AF.Exp, scale=-1.0)
        c.actv(d2[:], d2[:], AF.Ln, bias=1.0)
        c.actv(xtok[:], pTRb[:, 0:640], AF.Copy)
        c.actv(dec[:], pGT[:, 0:256].re("p (a b) -> p a b", b=128)[:, :, 63::64], AF.Exp)
        c.actv(eGe[:], pGT[:, 256:512], AF.Exp)
        pump()
        c.stt(dtt[:], d1[:], 0.0, d2[:], ALU.max, ALU.add)
        c.ts(dtt[:], dtt[:], maskt[:, t:t + 1], None, ALU.mult)
        c.tt(at[:], dtt[:], Abc[:], ALU.mult)
        c.tt(kend[:], ktok[:], eGe[:], ALU.mult)
        c.tt(Xdt[:].re("p (a b) -> p a b", b=64), xtok[:, 0:512].re("p (a b) -> p a b", b=64),
             dtt[:].us(2).bc([128, 8, 64]), ALU.mult)
        pump()
        pST0 = [banks[4], banks[7]]
        for h in range(4):
            hp, hh = divmod(h, 2)
            c.mm(pST0[hh][hh * 64:(hh + 1) * 64, hp * 128:(hp + 1) * 128], kend[0:64, h * 64:(h + 1) * 64],
                 vsb[0:64, h * 128:(h + 1) * 128])
        pCU = banks[3]
        c.mm(pCU[:, 0:8], TriIncl, at[:])
        c.mm(pCU[:, 8:16], StrictGT, at[:])
        c.mm(pCU[:, 16:24], Ones, at[:])
        pump()
        for h in range(4):
            hp, hh = divmod(h, 2)
            rows = slice(hh * 64, hh * 64 + 64)
            c.stt(S32[rows, hp, :], S32[rows, hp, :], dec[rows, hp, 0:1], pST0[hh][rows, hp * 128:(hp + 1) * 128],
                  ALU.mult, ALU.add)
        c.actv(ec[:], pCU[:, 0:24], AF.Exp)
        c.tt(Xde[:].re("p (a b) -> p a b", b=64), Xdt[:].re("p (a b) -> p a b", b=64),
             ec[:, 8:16].us(2).bc([128, 8, 64]), ALU.mult)
        pST1 = [banks[5], banks[6]]
        for h in range(4):
            hp, hh = divmod(h, 2)
            c.mm(pST1[hh][hh * 64:(hh + 1) * 64, hp * 128:(hp + 1) * 128], kend[64:128, h * 64:(h + 1) * 64],
                 vsb[64:128, h * 128:(h + 1) * 128])
        pSS = [banks[4], banks[7]]
        for g in range(2):
            c.mm(pSS[g][g * 64:(g + 1) * 64, 0:256], xtok[:, 512 + g * 64:512 + (g + 1) * 64], Xde[:, g * 256:(g + 1) * 256])
        pump()
        for h in range(4):
            hp, hh = divmod(h, 2)
            rows = slice(hh * 64, hh * 64 + 64)
            c.stt(S32[rows, hp, :], S32[rows, hp, :], dec[rows, hp, 1:2], pST1[hh][rows, hp * 128:(hp + 1) * 128],
                  ALU.mult, ALU.add)
        c.copy(Sb[0][:], S32[:], eng=c.pool)
        c.copy(dsel[0:64, :], ec[0:64, 16:20], eng=c.pool)
        c.copy(dsel[64:128, :], ec[64:128, 20:24], eng=c.pool)
        pump()
        c.tt(Ss32[:].re("p (a b) -> p a b", b=64), Ss32[:].re("p (a b) -> p a b", b=64),
             dsel[:].us(2).bc([128, 4, 64]), ALU.mult)
        for g in range(2):
            c.tt(Ss32[g * 64:(g + 1) * 64, :], Ss32[g * 64:(g + 1) * 64, :], pSS[g][g * 64:(g + 1) * 64, 0:256], ALU.add)
        c.copy(Ssb[:], Ss32[:], eng=c.pool)
        pump()

    def stage2(t, pump):
        own = t >= NPRE
        ot = t - NPRE
        par = t % 2
        xt = xb[par]
        lr1 = lr12[par]; vsb = vsb2[par]; gsil = gsil2[par]; zsil = zsil2[par]; xr = xr2[par]
        qks = qks2[par]; ktok = ktok2[par]; d1 = d12[par]
        cp(101)
        pZG = banks[3]
        c.mm(pZG[:, 0:256], lr1[0:17, :], wg2b[0:17, :])
        c.actv(Lt[:], pZG[:, 0:256], AF.Exp, scale=-1.0)
        c.actv(Lt[:], Lt[:], AF.Ln, bias=1.0)
        pump()
        pGT = banks[4]
        for hp in range(2):
            c.mm(pGT[:, hp * 128:(hp + 1) * 128], Lt[:, hp * 128:(hp + 1) * 128], TriNeg)
        c.mm(pGT[:, 256:512], StrictNeg, Lt[:, 0:256])
        c.actv(dec[:], pGT[:, 0:256].re("p (a b) -> p a b", b=128)[:, :, 63::64], AF.Exp)
        c.actv(eGe[:], pGT[:, 256:512], AF.Exp)
        c.tt(kend[:], ktok[:], eGe[:], ALU.mult)
        if own:
            c.actv(eG[:], pGT[:, 0:256], AF.Exp)
            c.actv(emG[:], pGT[:, 0:256], AF.Exp, scale=-1.0)
            c.stt(qTin[:].re("p a b -> p (a b)"), qks[:, 0:256], 0.125, eG[:], ALU.mult, ALU.mult)
            c.tt(kTin[:].re("p a b -> p (a b)"), qks[:, 256:512], emG[:], ALU.mult)
        pump()
        cp(102)
        if own:
            pAT = [banks[3], banks[5]]
            for h in range(4):
                hp, hh = divmod(h, 2)
                rows = slice(hh * 64, hh * 64 + 64)
                c.mm(pAT[hh][:, hp * 128:(hp + 1) * 128], kTin[rows, hp, :], qTin[rows, hp, :])
            for hh in range(2):
                c.tt(attm[:, hh::2, :], pAT[hh][:, 0:256].re("p (a b) -> p a b", b=128), gmask.us(1).bc([128, 2, 128]), ALU.mult)
            pump()
            pO = [banks[6], banks[7]]
            og3 = og[:].re("p (a b) -> p a b", b=128)
            for h in range(4):
                hp, hh = divmod(h, 2)
                c.mm(pO[hh][:, hp * 128:(hp + 1) * 128], attm[:, h, :], vsb[:, h * 128:(h + 1) * 128])
            for hh in range(2):
                c.actv(og3[:, hh::2, :], pO[hh][:, 0:256].re("p (a b) -> p a b", b=128), AF.Copy)
            pOb = [banks[3], banks[5]]
        cp(103)
        for cc in range(2):
            trows = slice(cc * 64, cc * 64 + 64)
            if own:
                for h in range(4):
                    hp, hh = divmod(h, 2)
                    rows = slice(hh * 64, hh * 64 + 64)
                    c.mm(pOb[hh][trows, hp * 128:(hp + 1) * 128], qTin[rows, hp, cc * 64:(cc + 1) * 64], Sb[cc][rows, hp, :])
            pST = [banks[6], banks[7]] if cc == 0 else [banks[4], banks[6]]
            for h in range(4):
                hp, hh = divmod(h, 2)
                c.mm(pST[hh][hh * 64:(hh + 1) * 64, hp * 128:(hp + 1) * 128], kend[trows, h * 64:(h + 1) * 64],
                     vsb[trows, h * 128:(h + 1) * 128])
            pump()
            for h in range(4):
                hp, hh = divmod(h, 2)
                rows = slice(hh * 64, hh * 64 + 64)
                c.stt(S32[rows, hp, :], S32[rows, hp, :], dec[rows, hp, cc:cc + 1], pST[hh][rows, hp * 128:(hp + 1) * 128],
                      ALU.mult, ALU.add)
            c.copy(Sb[1 - cc][:], S32[:], eng=c.pool)
        cp(104)
        if own:
            c.memset(gs[:], 0.0)
            for hh in range(2):
                c.tt(og3[:, hh::2, :], og3[:, hh::2, :], pOb[hh][:, 0:256].re("p (a b) -> p a b", b=128), ALU.add)
            for h in range(4):
                c.actv(junk[:, 0:128], og[:, h * 128:(h + 1) * 128], AF.Square, accum=gs[:, h:h + 1])
            c.ts(gs[:, 4:8], gs[:, 0:4], 1.0 / 128, EPS, ALU.mult, ALU.add)
            c.actv(gs[:, 4:8], gs[:, 4:8], AF.Ln)
            c.actv(gs[:, 4:8], gs[:, 4:8], AF.Exp, scale=-0.5)
            c.tt(og3, og3, gs[:, 4:8].us(2).bc([128, 4, 128]), ALU.mult)
            c.tt(og[:], og[:], gnw, ALU.mult)
            c.tt(mixed[:, 0:512], og[:], gsil[:], ALU.mult)
        cp(105)
        pCV = [banks[3], banks[5]]
        for i in range(6):
            dst = pCV[0][:, i * 128:(i + 1) * 128] if i < 4 else pCV[1][:, (i - 4) * 128:(i - 3) * 128]
            for k in range(4):
                c.mm(dst, diagW[:, i, k, :], xr[:, i, k:k + 128], start=(k == 0), stop=False)
            c.mm(dst, diagW[:, i, 4, :], ones_b[:], start=False, stop=True)
        c.actv(xc[:, 0:4, :].re("p a b -> p (a b)"), pCV[0][:], AF.Silu)
        c.actv(xc[:, 4:6, :].re("p a b -> p (a b)"), pCV[1][:, 0:256], AF.Silu)
        pump()
        pTRb = banks[0][:].bitcast(BF16)
        for i in range(5):
            c.tr(pTRb[:, i * 128:(i + 1) * 128], xc[:, i, :], ident_b[:])
        c.actv(xtok[:], pTRb[:, 0:640], AF.Copy)
        c.stt(d2[:], d1[:], -1.0, d1[:], ALU.mult, ALU.max)
        c.actv(d2[:], d2[:], AF.Exp, scale=-1.0)
        c.actv(d2[:], d2[:], AF.Ln, bias=1.0)
        c.stt(dtt[:], d1[:], 0.0, d2[:], ALU.max, ALU.add)
        if not own:
            c.ts(dtt[:], dtt[:], maskt[:, t:t + 1], None, ALU.mult)
        c.tt(at[:], dtt[:], Abc[:], ALU.mult)
        c.tt(Xdt[:].re("p (a b) -> p a b", b=64), xtok[:, 0:512].re("p (a b) -> p a b", b=64),
             dtt[:].us(2).bc([128, 8, 64]), ALU.mult)
        pCU = banks[4]
        c.mm(pCU[:, 0:8], TriIncl, at[:])
        c.mm(pCU[:, 8:16], StrictGT, at[:])
        c.mm(pCU[:, 16:24], Ones, at[:])
        c.actv(ec[:], pCU[:, 0:24], AF.Exp)
        pump()
        c.tt(Xde[:].re("p (a b) -> p a b", b=64), Xdt[:].re("p (a b) -> p a b", b=64),
             ec[:, 8:16].us(2).bc([128, 8, 64]), ALU.mult)
        if own:
            pSC = [banks[3], banks[5]]
            for g in range(2):
                rows = slice(g * 64, g * 64 + 64)
                c.mm(pSC[g][:, 0:128], xc[rows, 4, :], xc[rows, 5, :])
            for g in range(2):
                c.tt(sm[:, g, :], pSC[g][:, 0:128], TriIncl, ALU.mult)
            c.tt(Rb[:], TriIncl.us(1).bc([128, 8, 128]), at[:].us(2).bc([128, 8, 128]), ALU.mult, eng=c.pool)
            pump()
            for half in range(2):
                pSG = banks[6 + half]
                c.mm(pSG[:], StrictGT, Rb[:, half * 4:(half + 1) * 4, :].re("p a b -> p (a b)"))
                c.actv(LT[:, half * 4:(half + 1) * 4, :].re("p a b -> p (a b)"), pSG[:], AF.Exp)
                c.tt(MT[:, half * 4:(half + 1) * 4, :], LT[:, half * 4:(half + 1) * 4, :],
                     sm[:, half:half + 1, :].bc([128, 4, 128]), ALU.mult)
            pYD = banks[4]
            for h in range(8):
                c.mm(pYD[:, h * 64:(h + 1) * 64], MT[:, h, :], Xdt[:, h * 64:(h + 1) * 64])
            pYO = [banks[3], banks[5]]
            for g in range(2):
                rows = slice(g * 64, g * 64 + 64)
                c.mm(pYO[g][:, 0:256], xc[rows, 5, :], Ssb[rows, :])
            pump()
            for g in range(2):
                c.tt(yb[:, g * 256:(g + 1) * 256].re("p (a b) -> p a b", b=64), pYO[g][:, 0:256].re("p (a b) -> p a b", b=64),
                     ec[:, g * 4:(g + 1) * 4].us(2).bc([128, 4, 64]), ALU.mult)
            c.tt(yb[:], yb[:], pYD[:], ALU.add)
            c.tt(ytmp[:].re("p (a b) -> p a b", b=64), xtok[:, 0:512].re("p (a b) -> p a b", b=64),
                 dsk.us(2).bc([128, 8, 64]), ALU.mult, eng=c.pool)
            c.tt(yb[:], yb[:], ytmp[:], ALU.add)
            c.tt(yb[:], yb[:], zsil[:], ALU.mult)
            c.memset(gs[:, 0:2], 0.0)
            for g in range(2):
                c.actv(junk[:, 0:256], yb[:, g * 256:(g + 1) * 256], AF.Square, accum=gs[:, g:g + 1])
            c.ts(gs[:, 2:4], gs[:, 0:2], 1.0 / 256, EPS, ALU.mult, ALU.add)
            c.actv(gs[:, 2:4], gs[:, 2:4], AF.Ln)
            c.actv(gs[:, 2:4], gs[:, 2:4], AF.Exp, scale=-0.5)
            c.tt(yb[:].re("p (a b) -> p a b", b=256), yb[:].re("p (a b) -> p a b", b=256),
                 gs[:, 2:4].us(2).bc([128, 2, 256]), ALU.mult)
            c.tt(mixed[:, 512:1024], yb[:], snw, ALU.mult)
        cp(106)
        pSS = [banks[6], banks[7]]
        for g in range(2):
            c.mm(pSS[g][g * 64:(g + 1) * 64, 0:256], xtok[:, 512 + g * 64:512 + (g + 1) * 64], Xde[:, g * 256:(g + 1) * 256])
        pump()
        c.copy(dsel[0:64, :], ec[0:64, 16:20], eng=c.pool)
        c.copy(dsel[64:128, :], ec[64:128, 20:24], eng=c.pool)
        c.tt(Ss32[:].re("p (a b) -> p a b", b=64), Ss32[:].re("p (a b) -> p a b", b=64),
             dsel[:].us(2).bc([128, 4, 64]), ALU.mult)
        for g in range(2):
            c.tt(Ss32[g * 64:(g + 1) * 64, :], Ss32[g * 64:(g + 1) * 64, :], pSS[g][g * 64:(g + 1) * 64, 0:256], ALU.add)
        c.copy(Ssb[:], Ss32[:], eng=c.pool)
        if own:
            if stage <= 1:
                c.dma(c.sp, dbg[ot * 128:(ot + 1) * 128, :], mixed[:])
            pMTb = banks[0][:].bitcast(BF16)
            for i in range(8):
                c.tr(pMTb[:, i * 128:(i + 1) * 128], mixed[:, i * 128:(i + 1) * 128], ident_b[:])
            c.actv(mT[:].re("p a b -> p (a b)"), pMTb, AF.Copy)
            pump()
            for half in range(2):
                pOP = [banks[3], banks[5]][half]
                for fc in range(8):
                    c.mm(pOP[:], mT[:, fc, :], Wout[:, fc, half * 512:(half + 1) * 512], start=(fc == 0), stop=(fc == 7))
                c.tt(hbuf[:, half * 512:(half + 1) * 512], xt[:, half * 512:(half + 1) * 512], pOP[:], ALU.add)
            c.dma(c.sp, hs[ot * 128:(ot + 1) * 128, :], hbuf[:])
            pump()
            rmsnorm_T(hbuf[:], nfw, n2T[:, :, ot * 128:(ot + 1) * 128], (ssR, junk, xsbR))

    gen = stage1(0)
    for _ in gen:
        pass
    cp(100)
    for t in range(NT):
        nxt = stage1(t + 1) if t + 1 < NT else iter(())
        (stage2_prefix if t < NPRE else stage2)(t, lambda: next(nxt, None))
        for _ in nxt:
            pass
    while conv_jobs and stage > 1:
        issue_conv()
    c.barrier()
    esA.close()
    if stage <= 1:
        c.finish(c._last_dma_tokens())
        c.es.close()
        return nc
    build_peer(nc, c, locals())
    return nc


def build_peer(nc, c, L):
    NOWN = L["NOWN"]; TOK = L["TOK"]; banks = L["banks"]; n2T = L["n2T"]; ident_b = L["ident_b"]
    w_q = L["w_q"]; skTd = L["skTd"]; uTd = L["uTd"]; vd = L["vd"]; hs = L["hs"]; y = L["y"]; fnw = L["fnw"]
    ident_f = L["ident_f"]; iota_f = L["iota_f"]; repd = L["repd"]; ubd = L["ubd"]; vbd = L["vbd"]
    V = nc.vector
    TB = min(2, NOWN)
    BT = TB * 128
    bnc = c.dram("bnc", [TOK, 24 * 128], BF16, "Internal")
    negv0T = c.sbuf("negv0T", [128, TOK], F32)
    idx0T = c.sbuf("idx0T", [128, TOK], BF16)
    e0T = c.sbuf("e0T", [128, TOK], F32)
    reps = c.sbuf("reps", [24, 2, 128], BF16)
    c.dma(c.pool, reps[:].re("p a b -> p (a b)"), repd[:])

    es = contextlib.ExitStack()
    Wq = c.sbuf("Wq", [128, 8, 2048], BF16, es=es)
    skT = c.sbuf("skT", [128, 16, 128], BF16, es=es)
    c.dma(c.pool, Wq[:], w_q[:].re("(c p) n -> p c n", p=128))
    c.dma(c.pool, skT[:].re("p a b -> p (a b)"), skTd[:])
    qTsb = c.sbuf("qTsb", [128, 16, 128], BF16, es=es)
    S = c.sbuf("S", [128, 16, 128], F32, es=es)
    s2 = c.sbuf("s2", [128, 128], F32, es=es)
    v16 = c.sbuf("v16", [128, 16, 16], F32, es=es)
    i16 = c.sbuf("i16", [128, 8, 16], U32, es=es)
    cand = c.sbuf("cand", [128, 8, 256], F32, es=es)
    cand2 = c.sbuf("cand2", [128, 256], F32, es=es)
    b16 = c.sbuf("b16", [128, 8, 16], F32, es=es)
    eb = c.sbuf("eb", [128, 8, 16], F32, es=es)
    zz = c.sbuf("zz", [128, 24], F32, es=es)
    tk = c.sbuf("tk", [128, 3, 128], F32, es=es)
    s1p = c.sbuf("s1p", [128, 8, 128], F32, es=es)
    e1f = c.sbuf("e1f", [128, 8, 128], F32, es=es)
    bt = [c.sbuf(f"bt{i}", [128, 8, 3, 128], BF16, es=es) for i in range(2)]
    Sd = [S, c.sbuf("S_b", [128, 16, 128], F32, es=es)]

    def front(ot):
        tok = slice(ot * 128, (ot + 1) * 128)
        for hp in range(16):
            pb = banks[hp // 4]
            for dc in range(8):
                c.mm(pb[:, (hp % 4) * 128:(hp % 4 + 1) * 128], Wq[:, dc, hp * 128:(hp + 1) * 128], n2T[:, dc, tok],
                     start=(dc == 0), stop=(dc == 7))
        for q4 in range(4):
            c.actv(qTsb[:, q4 * 4:(q4 + 1) * 4, :].re("p a b -> p (a b)"), banks[q4][:], AF.Copy)
        for hp in range(16):
            c.mm(banks[4 + hp // 4][:, (hp % 4) * 128:(hp % 4 + 1) * 128], qTsb[:, hp, :], skT[:, hp, :])
        for q4 in range(4):
            c.actv(Sd[ot % 2][:, q4 * 4:(q4 + 1) * 4, :].re("p a b -> p (a b)"), banks[4 + q4][:], AF.Copy)

    front(0)
    for ot in range(NOWN):
        tok = slice(ot * 128, (ot + 1) * 128)
        S = Sd[ot % 2]
        if ot + 1 < NOWN:
            front(ot + 1)
        for hp in range(16):
            h, p = divmod(hp, 2)
            c.op(c.dve, lambda hp=hp: V.max(out=v16.ap[:, hp, 0:8], in_=S.ap[:, hp, :]), [S], [v16])
            if p == 0:
                c.op(c.dve, lambda hp=hp, h=h: V.max_index(out=i16.ap[:, h, 0:8], in_max=v16.ap[:, hp, 0:8], in_values=S.ap[:, hp, :]),
                     [S, v16], [i16])
            c.op(c.dve, lambda hp=hp: V.match_replace(out=s2.ap, in_to_replace=v16.ap[:, hp, 0:8], in_values=S.ap[:, hp, :],
                                                      imm_value=-1e30), [S, v16], [s2])
            c.op(c.dve, lambda hp=hp: V.max(out=v16.ap[:, hp, 8:16], in_=s2.ap), [s2], [v16])
            if p == 0:
                c.op(c.dve, lambda hp=hp, h=h: V.max_index(out=i16.ap[:, h, 8:16], in_max=v16.ap[:, hp, 8:16], in_values=s2.ap),
                     [s2, v16], [i16])
        v16v = v16[:].re("p (h two) r -> p h two r", two=2)
        c.tt(cand[:].re("p h (a b) -> p h a b", b=16), v16v[:, :, 0, :].us(3).bc([128, 8, 16, 16]),
             v16v[:, :, 1, :].us(2).bc([128, 8, 16, 16]), ALU.add)
        for h in range(8):
            c.op(c.dve, lambda h=h: V.max(out=b16.ap[:, h, 0:8], in_=cand.ap[:, h, :]), [cand], [b16])
            c.op(c.dve, lambda h=h: V.match_replace(out=cand2.ap, in_to_replace=b16.ap[:, h, 0:8], in_values=cand.ap[:, h, :],
                                                    imm_value=-1e30), [cand, b16], [cand2])
            c.op(c.dve, lambda h=h: V.max(out=b16.ap[:, h, 8:16], in_=cand2.ap), [cand2], [b16])
        c.tt(eb[:], b16[:], b16[:, :, 0:1].bc([128, 8, 16]), ALU.subtract)
        c.actv(eb[:], eb[:], AF.Exp)
        c.reduce(zz[:, 0:8], eb[:], ALU.add)
        c.op(c.dve, lambda: V.reciprocal(out=zz.ap[:, 8:16], in_=zz.ap[:, 0:8]), [zz], [zz])
        c.ts(zz[:, 16:24], b16[:, :, 15], -2e-4, None, ALU.add)
        tk3 = tk[:].re("p a (h r) -> p a h r", r=16)
        c.ts(tk3[:, 0], v16v[:, :, 0, :], -1.0, None, ALU.mult)
        c.copy(tk3[:, 1], i16[:])
        c.tt(tk3[:, 2], v16v[:, :, 0, :], v16v[:, :, 0, 0:1].bc([128, 8, 16]), ALU.subtract)
        c.actv(tk3[:, 2], tk3[:, 2], AF.Exp)
        c.tt(tk3[:, 2], tk3[:, 2], zz[:, 8:16].us(2).bc([128, 8, 16]), ALU.mult)
        pK = banks[ot % 2]
        for a in range(3):
            c.tr(pK[:, a * 128:(a + 1) * 128], tk[:, a, :], ident_f)
        c.actv(negv0T[:, tok], pK[:, 0:128], AF.Copy)
        c.actv(idx0T[:, tok], pK[:, 128:256], AF.Copy)
        c.actv(e0T[:, tok], pK[:, 256:384], AF.Copy)
        Sv = S[:].re("p (h two) k -> p h two k", two=2)
        B = bt[ot % 2]
        c.tt(s1p[:], Sv[:, :, 1, :], zz[:, 16:24].us(2).bc([128, 8, 128]), ALU.subtract)
        c.copy(B[:, :, 0, :], s1p[:])
        c.tt(B[:, :, 1, :], s1p[:], B[:, :, 0, :], ALU.subtract)
        c.tt(e1f[:], Sv[:, :, 1, :], v16v[:, :, 1, 0:1].bc([128, 8, 128]), ALU.subtract)
        c.actv(B[:, :, 2, :], e1f[:], AF.Exp)
        c.dma(c.sp, bnc[tok, :], B[:].re("p h a j -> p (h a j)"))
    c.barrier()
    es.close()

    es = contextlib.ExitStack()
    GT = c.sbuf("GT", [128, BT, 128], BF16, es=es)
    RG = 32
    rbuf = [c.sbuf(f"rbuf{i}", [24, RG, 128], BF16, es=es) for i in range(2)]
    e1rep = [c.sbuf(f"e1rep{i}", [128, 512], BF16, es=es) for i in range(2)]
    NR = 8
    rowb = [c.sbuf(f"rowb{i}", [128, 128], BF16, es=es) for i in range(NR)]
    oh4b = [c.sbuf(f"oh4b{i}", [128, 4, 128], BF16, es=es) for i in range(3)]
    ub = [c.sbuf(f"ub{i}", [128, 8, 512], BF16, es=es) for i in range(2)]
    vb = [c.sbuf(f"vb{i}", [128, 4, 1024], BF16, es=es) for i in range(2)]
    NH = 3
    gel = [c.sbuf(f"gel{i}", [128, BT], BF16, es=es) for i in range(NH)]
    hidT = [c.sbuf(f"hidT{i}", [128, BT], BF16, es=es) for i in range(NH)]
    hb = c.sbuf("hb", [128, 1024], F32, es=es)
    ob = c.sbuf("ob", [128, 1024], F32, es=es)
    fs = c.sbuf("fs", [128, 4], F32, es=es)
    bnc3 = bnc[:].re("t (q j) -> t q j", j=128)
    tokctr = 0
    for tb in range(NOWN // TB):
        t0 = tb * BT
        groups = [(rg, q4) for rg in range(BT // RG) for q4 in range(RG // 4)]

        def g_stage1(n):
            rg, q4 = groups[n]
            rb = rbuf[rg % 2]
            if q4 == 0:
                c.dma(c.sp, rb[:], bnc3[t0 + rg * RG:t0 + (rg + 1) * RG].re("t q j -> q t j"))
            par = n % 2
            pS = banks[0 + par]; pE = banks[2 + par]
            rhs = rb[:, q4 * 4:(q4 + 1) * 4, :].re("p a b -> p (a b)")
            c.mm(pS[:], reps[:, 0, :], rhs)
            c.mm(pE[:], reps[:, 1, :], rhs)
            lt1 = rg * RG + q4 * 4
            for u in range(4):
                gt1 = t0 + lt1 + u
                c.actv(e1rep[par][:, u * 128:(u + 1) * 128], pE[:, u * 128:(u + 1) * 128], AF.Copy, scale=e0T[:, gt1:gt1 + 1])

        def g_stage2(n):
            nonlocal tokctr
            rg, q4 = groups[n]
            lt = rg * RG + q4 * 4
            par = n % 2
            pS = banks[0 + par]; pG = banks[4 + par]
            er = e1rep[par]
            oh4 = oh4b[n % 3]
            gt0 = t0 + lt
            c.stt(oh4[:], iota_f.us(1).bc([128, 4, 128]), 0.0, idx0T[:, gt0:gt0 + 4].us(2).bc([128, 4, 128]),
                  ALU.add, ALU.is_equal)
            for u in range(4):
                gt = gt0 + u
                rw = rowb[tokctr % NR]
                tokctr += 1
                c.stt(rw[:], pS[:, u * 128:(u + 1) * 128], negv0T[:, gt:gt + 1], er[:, u * 128:(u + 1) * 128],
                      ALU.is_ge, ALU.mult)
                c.mm(pG[:, u * 128:(u + 1) * 128], rw[:], oh4[:, u, :])
            c.actv(GT[:, lt:lt + 4, :].re("p a b -> p (a b)"), pG[:], AF.Copy)

        g_stage1(0)
        for n in range(len(groups)):
            if n + 1 < len(groups):
                g_stage1(n + 1)
            g_stage2(n)
        pOut = [[banks[4], banks[5]], [banks[6], banks[7]]]
        chunks = [(g, ci) for g in range(NG) for ci in range(4)]

        def emit_act(n):
            g, ci = chunks[n]
            k = g % 2
            if ci == 0:
                c.dma(c.sp, ub[k][:], ubd[g][:].re("(c p) e -> p c e", p=128))
                c.dma(c.sp, vb[k][:], vbd[g][:].re("(c p) d -> p c d", p=128))
            pA = banks[n % 2]
            for dc in range(8):
                c.mm(pA[:, 0:BT], ub[k][:, dc, ci * 128:(ci + 1) * 128], n2T[:, dc, t0:t0 + BT], start=(dc == 0), stop=(dc == 7))
            c.actv(gel[n % NH][:], pA[:, 0:BT], AF.Gelu)
            c.tt(hidT[n % NH][:], gel[n % NH][:], GT[:, :, g * 4 + ci], ALU.mult)

        def emit_out(n):
            g, ci = chunks[n]
            k = g % 2
            for tt in range(TB):
                for half in range(2):
                    c.mm(pOut[tt][half][:], hidT[n % NH][:, tt * 128:(tt + 1) * 128], vb[k][:, ci, half * 512:(half + 1) * 512],
                         start=(n == 0), stop=(n == len(chunks) - 1))

        emit_act(0)
        for n in range(len(chunks)):
            if n + 1 < len(chunks):
                emit_act(n + 1)
            emit_out(n)
        for tl in range(TB):
            ot = tb * TB + tl
            c.dma(c.sp, hb[:], hs[ot * 128:(ot + 1) * 128, :])
            for half in range(2):
                c.tt(hb[:, half * 512:(half + 1) * 512], hb[:, half * 512:(half + 1) * 512], pOut[tl][half][:], ALU.add)
            c.memset(fs[:, 0:1], 0.0)
            c.actv(ob[:], hb[:], AF.Square, accum=fs[:, 0:1])
            c.ts(fs[:, 1:2], fs[:, 0:1], 1.0 / D, EPS, ALU.mult, ALU.add)
            c.actv(fs[:, 2:3], fs[:, 1:2], AF.Ln)
            c.actv(fs[:, 3:4], fs[:, 2:3], AF.Exp, scale=-0.5)
            c.stt(ob[:], hb[:], fs[:, 3:4], fnw, ALU.mult, ALU.mult)
            c.dma(c.sp, y[ot * 128:(ot + 1) * 128, :], ob[:])
    c.barrier()
    c.finish(c._last_dma_tokens())
    es.close()
    c.es.close()

def make_consts():
    s = np.arange(128)[:, None]; t = np.arange(128)[None, :]
    same = (s // 64) == (t // 64)
    ident = (s == t).astype(np.float32)
    TriNeg = np.where((s <= t) & same, -1.0 / 16, 0.0).astype(np.float32)
    StrictNeg = np.where((s > t) & same, -1.0 / 16, 0.0).astype(np.float32)
    gmask = ((t >= s) & same).astype(np.float32)
    TriIncl = (s <= t).astype(np.float32)
    StrictGT = (s > t).astype(np.float32)
    Ones = np.ones((128, 128), np.float32)
    iota = np.broadcast_to(t.astype(np.float32), (128, 128))
    return np.ascontiguousarray(np.concatenate([ident, TriNeg, StrictNeg, gmask, TriIncl, StrictGT, Ones, iota], axis=1))


def prep_shared(inp):
    f = lambda a: np.ascontiguousarray(np.asarray(a, dtype=np.float32))
    pp = np.zeros((128, 46), np.float32)
    pp[:, 0:8] = f(inp["norm_mix_w"])[0].reshape(8, 128).T
    pp[:, 8:16] = f(inp["norm_ffn_w"])[0].reshape(8, 128).T
    cw = f(inp["ssd_conv_w"])[0]
    pp[:, 16:40] = cw.reshape(4, 6, 128).transpose(2, 1, 0).reshape(128, 24)
    pp[:, 40:46] = f(inp["ssd_conv_b"])[0].reshape(6, 128).T
    bc = np.zeros((128, 24 + 512 + 512 + 1024), np.float32)
    bc[:, 0:8] = f(inp["ssd_dt_bias"])[0][None]
    bc[:, 8:16] = f(inp["ssd_a_log"])[0][None]
    bc[:, 16:24] = f(inp["ssd_d"])[0][None]
    bc[:, 24:536] = np.tile(f(inp["gla_norm_w"])[0], 4)[None]
    bc[:, 536:1048] = f(inp["ssd_norm_w"])[0][None]
    bc[:, 1048:2072] = f(inp["norm_final_w"])[None]
    wg2b = np.concatenate([f(inp["gla_w_gate2"])[0], f(inp["gla_b_gate"])[0][None]], axis=0)
    sk = f(inp["peer_sub_keys"])[0]
    skT = np.ascontiguousarray(sk.transpose(3, 0, 1, 2).reshape(128, 16 * 128))
    rep = np.zeros((24, 2, 128), np.float32)
    for q in range(24):
        hh, a = divmod(q, 3)
        rep[q, 0 if a < 2 else 1, hh * 16:(hh + 1) * 16] = 1.0
    return {
        "repd": np.ascontiguousarray(rep.reshape(24, 256)),
        "constd": make_consts(), "ppd": pp, "bcd": bc, "wg2bd": np.ascontiguousarray(wg2b),
        "w_in": f(inp["w_in"])[0], "w_out": f(inp["w_out"])[0], "w_q": f(inp["peer_w_query"])[0],
        "skTd": skT, "uTd": np.ascontiguousarray(f(inp["peer_u"])[0].T), "vd": f(inp["peer_v"])[0],
    }


def kernel(**inputs):
    x = np.asarray(inputs["x"], dtype=np.float32)
    sh = prep_shared(inputs)
    NPRE, NOWN = 48, 16
    in_maps = []
    for core in range(8):
        b, j = divmod(core, 4)
        xin = np.zeros((64 * 128, 1024), np.float32)
        mask = np.zeros((128, 64), np.float32)
        lo = (j - 3) * 2048
        hi = (j + 1) * 2048
        src_lo = max(lo, 0)
        n = hi - src_lo
        xin[64 * 128 - n:] = x[b, src_lo:hi]
        mask[:, 64 - n // 128:] = 1.0
        m = dict(sh)
        m["xin"] = xin
        m["maskd"] = mask
        in_maps.append(m)
    nc = build_program(NPRE, NOWN, stage=99)
    res = run_bass_kernel_spmd(nc, in_maps, core_ids=list(range(8)))
    out = np.zeros((2, 8192, 1024), np.float32)
    for core in range(8):
        b, j = divmod(core, 4)
        out[b, j * 2048:(j + 1) * 2048] = res.results[core]["y"]
    return out
```
